# Optimizing a Trainium2 kernel written in Bass

```python
import math
import jax, jax.numpy as jnp
from jax import lax
import numpy as np

D_MODEL = 1024
BATCH = 2
SEQ = 8192
DEPTH = 4

GRID_W = 64
CTX_LEN = 256
N_MIXERS = 3
NORM_EPS = 1e-6
DA_HEADS = 8
DA_HEAD_DIM = 64
DA_QBLOCK = 128
ROPE_BASE = 10000.0
SSM_D_INNER = 2 * D_MODEL
SSM_HEAD_DIM = 64
SSM_HEADS = SSM_D_INNER // SSM_HEAD_DIM
SSM_GROUPS = 4
SSM_STATE = 128
SSM_CONV = 3
SSM_CHUNK = 128
SSM_CONV_DIM = SSM_D_INNER + 2 * SSM_GROUPS * SSM_STATE
SSM_IN_DIM = SSM_D_INNER + SSM_CONV_DIM + 2 * SSM_HEADS
NA_HEADS = 16
NA_HEAD_DIM = D_MODEL // NA_HEADS
NA_ROWS = 8
NA_COLS = 16
N_EXPERTS = 16
EC_CAPACITY_FACTOR = 2
EXPERT_FF = 2 * D_MODEL
N_A_LAYERS = (DEPTH + 2) // 3
N_B_LAYERS = (DEPTH + 1) // 3
N_C_LAYERS = DEPTH // 3

kernel_name = "hybrid_diffattn_ssd_natten_ecmoe_trunk"

F32 = jnp.float32


def rms_norm(x, w):
    xf = x.astype(F32)
    y = xf * lax.rsqrt(jnp.mean(xf * xf, axis=-1, keepdims=True) + NORM_EPS)
    return (y * w.astype(F32)).astype(x.dtype)


def modulate(h, shift, scale):
    return h * (1 + scale) + shift


def lambda_init(layer):
    return 0.8 - 0.6 * math.exp(-0.3 * layer)


def dense_attention(q, k, v, scale):
    s = jnp.einsum("bqhd,bkhd->bhqk", q, k).astype(F32) * scale
    p = jax.nn.softmax(s, axis=-1).astype(v.dtype)
    return jnp.einsum("bhqk,bkhd->bqhd", p, v)


def axial_rope_tables(n_tokens, dim, dtype):
    t = jnp.arange(n_tokens)
    rows = (t // GRID_W).astype(F32)
    cols = (t % GRID_W).astype(F32)
    quarter = dim // 4
    freqs = ROPE_BASE ** (-jnp.arange(quarter, dtype=F32) / quarter)
    ang_r = rows[:, None] * freqs[None, :]
    ang_c = cols[:, None] * freqs[None, :]
    ang = jnp.concatenate([ang_r, ang_r, ang_c, ang_c], axis=-1)
    return jnp.cos(ang).astype(dtype), jnp.sin(ang).astype(dtype)


def apply_axial_rope(x, cos, sin):
    d = x.shape[-1]
    xs = x.reshape(x.shape[:-1] + (2, 2, d // 4))
    rot = jnp.stack([-xs[..., 1, :], xs[..., 0, :]], axis=-2).reshape(x.shape)
    return x * cos[:, None, None, :] + rot * sin[:, None, None, :]


def diff_attention(h_ctx, h_lat, wqkv, wo, lq1, lk1, lq2, lk2, subln, lam_init, need_ctx):
    H, d = DA_HEADS, DA_HEAD_DIM
    scale = d ** -0.5

    def project(h):
        b, n, _ = h.shape
        q, k, v = jnp.split(h @ wqkv, [H * 2 * d, 2 * H * 2 * d], axis=-1)
        return q.reshape(b, n, H, 2, d), k.reshape(b, n, H, 2, d), v.reshape(b, n, H, 2 * d)

    q_c, k_c, v_c = project(h_ctx)
    q_l, k_l, v_l = project(h_lat)
    b, s = h_lat.shape[:2]
    cos, sin = axial_rope_tables(s, d, h_lat.dtype)
    q_l = apply_axial_rope(q_l, cos, sin)
    k_l = apply_axial_rope(k_l, cos, sin)
    lam = (jnp.exp(jnp.sum(lq1.astype(F32) * lk1.astype(F32)))
           - jnp.exp(jnp.sum(lq2.astype(F32) * lk2.astype(F32))) + lam_init)

    def attend(q, k, v):
        sc = jnp.einsum("bqhmd,bkhmd->bhmqk", q, k).astype(F32) * scale
        p = jax.nn.softmax(sc, axis=-1)
        w = (p[:, :, 0] - lam * p[:, :, 1]).astype(v.dtype)
        return jnp.einsum("bhqk,bkhe->bqhe", w, v)

    def post(o):
        o = rms_norm(o, subln) * (1 - lam_init)
        return o.reshape(o.shape[0], o.shape[1], H * 2 * d) @ wo

    k_all = jnp.concatenate([k_c, k_l], axis=1)
    v_all = jnp.concatenate([v_c, v_l], axis=1)
    nb = s // DA_QBLOCK
    q_blocks = jnp.moveaxis(q_l.reshape(b, nb, DA_QBLOCK, H, 2, d), 1, 0)
    o_l = lax.map(lambda qb: attend(qb, k_all, v_all), q_blocks)
    o_l = jnp.moveaxis(o_l, 0, 1).reshape(b, s, H, 2 * d)
    out_l = post(o_l)
    out_c = post(attend(q_c, k_c, v_c)) if need_ctx else None
    return out_c, out_l


def centred_depthwise_conv(u, w, bias):
    k = w.shape[0]
    out = lax.conv_general_dilated(u, w[:, None, :], window_strides=(1,),
                                   padding=[((k - 1) // 2, k // 2)],
                                   dimension_numbers=("NWC", "WIO", "NWC"),
                                   feature_group_count=u.shape[-1])
    return out + bias


def ssd_chunked(x, dt, Bm, Cm, A, h0):
    b, l, nh, p = x.shape
    g, n = Bm.shape[-2:]
    r = nh // g
    L = SSM_CHUNK
    nc = l // L
    xd = (x.astype(F32) * dt[..., None]).reshape(b, nc, L, g, r, p)
    a = (dt * A).reshape(b, nc, L, g, r)
    acs = jnp.cumsum(a, axis=2)
    Bc = Bm.astype(F32).reshape(b, nc, L, g, n)
    Cc = Cm.astype(F32).reshape(b, nc, L, g, n)
    lower = jnp.tril(jnp.ones((L, L), dtype=bool))
    seg = acs[:, :, :, None] - acs[:, :, None, :]
    decay = jnp.exp(jnp.where(lower[:, :, None, None], seg, -jnp.inf))
    cb = jnp.einsum("bclgn,bcsgn->bclsg", Cc, Bc)
    y_diag = jnp.einsum("bclsgr,bcsgrp->bclgrp", cb[..., None] * decay, xd)
    decay_to_end = jnp.exp(acs[:, :, -1:] - acs)
    chunk_states = jnp.einsum("bclgn,bclgrp->bcgrpn", Bc, xd * decay_to_end[..., None])
    chunk_decay = jnp.exp(acs[:, :, -1])

    def step(state, inp):
        dec, st = inp
        return state * dec[..., None, None] + st, state

    h_final, h_prev = lax.scan(step, h0, (jnp.moveaxis(chunk_decay, 1, 0),
                                          jnp.moveaxis(chunk_states, 1, 0)))
    h_prev = jnp.moveaxis(h_prev, 0, 1)
    y_off = jnp.einsum("bclgn,bcgrpn->bclgrp", Cc, h_prev) * jnp.exp(acs)[..., None]
    return (y_diag + y_off).reshape(b, l, nh, p), h_final


def mamba2_mixer(h_ctx, h_lat, w_in, conv_w, conv_b, dt_bias, A_log, d_skip, norm_w, w_out, need_ctx):
    NH, P, G, N = SSM_HEADS, SSM_HEAD_DIM, SSM_GROUPS, SSM_STATE

    def in_proj(h):
        b, n, _ = h.shape
        z, xbc, dt_raw = jnp.split(h @ w_in, [SSM_D_INNER, SSM_D_INNER + SSM_CONV_DIM], axis=-1)
        xbc = jax.nn.silu(centred_depthwise_conv(xbc, conv_w, conv_b))
        xs, Bm, Cm = jnp.split(xbc, [SSM_D_INNER, SSM_D_INNER + G * N], axis=-1)
        return (z, xs.reshape(b, n, NH, P), Bm.reshape(b, n, G, N), Cm.reshape(b, n, G, N),
                dt_raw.reshape(b, n, 2, NH))

    zc, xc, Bc, Cc, dtc = in_proj(h_ctx)
    zl, xl, Bl, Cl, dtl = in_proj(h_lat)
    b = h_lat.shape[0]
    y_c = jnp.zeros(xc.shape, F32)
    y_l = jnp.zeros(xl.shape, F32)
    for direction in range(2):
        A = -jnp.exp(A_log[direction].astype(F32))
        dskip = d_skip[direction].astype(F32)[:, None]

        def prep(xs, Bm, Cm, dt_raw):
            dt = jax.nn.softplus((dt_raw[:, :, direction] + dt_bias[direction]).astype(F32))
            seqs = (xs, dt, Bm, Cm)
            if direction == 1:
                seqs = tuple(jnp.flip(t, axis=1) for t in seqs)
            return seqs

        h0 = jnp.zeros((b, G, NH // G, P, N), F32)
        xs_c, dt_c, B_c, C_c = prep(xc, Bc, Cc, dtc)
        yd_c, h_ctx_final = ssd_chunked(xs_c, dt_c, B_c, C_c, A, h0)
        xs_l, dt_l, B_l, C_l = prep(xl, Bl, Cl, dtl)
        yd_l, _ = ssd_chunked(xs_l, dt_l, B_l, C_l, A, h_ctx_final)
        if direction == 1:
            yd_c = jnp.flip(yd_c, axis=1)
            yd_l = jnp.flip(yd_l, axis=1)
        y_c = y_c + yd_c + dskip * xc.astype(F32)
        y_l = y_l + yd_l + dskip * xl.astype(F32)

    def post(y, z):
        bb, n = y.shape[:2]
        y = y.reshape(bb, n, SSM_D_INNER).astype(z.dtype)
        return rms_norm(y * jax.nn.silu(z), norm_w) @ w_out

    out_l = post(y_l, zl)
    out_c = post(y_c, zc) if need_ctx else None
    return out_c, out_l


def neighbourhood_attention(h_ctx, h_lat, wqkv, wo, rpb, need_ctx):
    H, d = NA_HEADS, NA_HEAD_DIM
    scale = d ** -0.5

    def project(h):
        b, n, _ = h.shape
        q, k, v = jnp.split(h @ wqkv, 3, axis=-1)
        return q.reshape(b, n, H, d), k.reshape(b, n, H, d), v.reshape(b, n, H, d)

    q_c, k_c, v_c = project(h_ctx)
    q_l, k_l, v_l = project(h_lat)
    b, s = h_lat.shape[:2]
    rows = s // GRID_W
    wr = min(NA_ROWS, rows)
    n_w = wr * NA_COLS
    k_g = k_l.reshape(b, rows, GRID_W, H, d)
    v_g = v_l.reshape(b, rows, GRID_W, H, d)
    qcol = jnp.arange(GRID_W)
    col_start = jnp.clip(qcol - NA_COLS // 2, 0, GRID_W - NA_COLS)
    col_idx = col_start[:, None] + jnp.arange(NA_COLS)[None, :]
    dc = col_idx - qcol[:, None] + NA_COLS - 1
    q_rows = jnp.moveaxis(q_l.reshape(b, rows, GRID_W, H, d), 1, 0)

    def row_block(args):
        r, q_r = args
        rs = jnp.clip(r - wr // 2, 0, rows - wr)
        k_w = lax.dynamic_slice_in_dim(k_g, rs, wr, axis=1)[:, :, col_idx]
        v_w = lax.dynamic_slice_in_dim(v_g, rs, wr, axis=1)[:, :, col_idx]
        dr = rs + jnp.arange(wr) - r + NA_ROWS - 1
        bias = rpb[:, dr[:, None, None], dc[None, :, :]].transpose(0, 2, 1, 3).astype(F32)
        s_w = jnp.einsum("bqhd,bwqjhd->bhqwj", q_r, k_w).astype(F32) * scale + bias[None]
        s_c = jnp.einsum("bqhd,bkhd->bhqk", q_r, k_c).astype(F32) * scale
        sc = jnp.concatenate([s_w.reshape(b, H, GRID_W, n_w), s_c], axis=-1)
        p = jax.nn.softmax(sc, axis=-1).astype(v_l.dtype)
        p_w = p[..., :n_w].reshape(b, H, GRID_W, wr, NA_COLS)
        return (jnp.einsum("bhqwj,bwqjhd->bqhd", p_w, v_w)
                + jnp.einsum("bhqk,bkhd->bqhd", p[..., n_w:], v_c))

    o_l = lax.map(row_block, (jnp.arange(rows), q_rows))
    out_l = jnp.moveaxis(o_l, 0, 1).reshape(b, s, H * d) @ wo
    out_c = None
    if need_ctx:
        o_c = dense_attention(q_c, k_c, v_c, scale)
        out_c = o_c.reshape(o_c.shape[0], o_c.shape[1], H * d) @ wo
    return out_c, out_l


def expert_choice_moe(h, router_w, w1, w3, w2):
    b, n, _ = h.shape
    cap = EC_CAPACITY_FACTOR * n // N_EXPERTS
    aff = jax.nn.softmax((h @ router_w).astype(F32), axis=-1)
    gate, idx = lax.top_k(jnp.swapaxes(aff, 1, 2), cap)
    bidx = jnp.arange(b)[:, None, None]
    xs = h[bidx, idx]
    hid = jax.nn.silu(jnp.einsum("becd,edf->becf", xs, w1)) * jnp.einsum("becd,edf->becf", xs, w3)
    y = jnp.einsum("becf,efd->becd", hid, w2)
    y = y * gate[..., None].astype(y.dtype)
    return jnp.zeros_like(h).at[bidx, idx].add(y.astype(h.dtype))


def setup_inputs(seed: int = 0) -> dict:
    key = jax.random.key(seed)
    ks = iter(jax.random.split(key, 48))

    def nrm(shape, scale):
        return jax.random.normal(next(ks), shape, F32) * scale

    D = D_MODEL
    qkv_a = 3 * DA_HEADS * 2 * DA_HEAD_DIM
    dt0 = jnp.exp(jax.random.uniform(next(ks), (N_B_LAYERS, 2, SSM_HEADS), F32,
                                     minval=math.log(1e-3), maxval=math.log(1e-1)))
    return {
        "x": nrm((BATCH, SEQ, D), 1.0),
        "c": nrm((BATCH, D), 1.0),
        "ctx": nrm((BATCH, CTX_LEN, D), 1.0),
        "c_ctx": nrm((D,), 1.0),
        "ada_w": nrm((DEPTH, D, 6 * D), 0.02),
        "ada_b": nrm((DEPTH, 6 * D), 0.01),
        "norm_mix": 1.0 + nrm((DEPTH, D), 0.02),
        "norm_ffn": 1.0 + nrm((DEPTH, D), 0.02),
        "final_norm": 1.0 + nrm((D,), 0.02),
        "da_wqkv": nrm((N_A_LAYERS, D, qkv_a), D ** -0.5),
        "da_wo": nrm((N_A_LAYERS, DA_HEADS * 2 * DA_HEAD_DIM, D), (DA_HEADS * 2 * DA_HEAD_DIM) ** -0.5),
        "da_lam_q1": nrm((N_A_LAYERS, DA_HEAD_DIM), 0.1),
        "da_lam_k1": nrm((N_A_LAYERS, DA_HEAD_DIM), 0.1),
        "da_lam_q2": nrm((N_A_LAYERS, DA_HEAD_DIM), 0.1),
        "da_lam_k2": nrm((N_A_LAYERS, DA_HEAD_DIM), 0.1),
        "da_subln": 1.0 + nrm((N_A_LAYERS, 2 * DA_HEAD_DIM), 0.02),
        "ssm_w_in": nrm((N_B_LAYERS, D, SSM_IN_DIM), D ** -0.5),
        "ssm_conv_w": nrm((N_B_LAYERS, SSM_CONV, SSM_CONV_DIM), SSM_CONV ** -0.5),
        "ssm_conv_b": nrm((N_B_LAYERS, SSM_CONV_DIM), 0.01),
        "ssm_dt_bias": dt0 + jnp.log(-jnp.expm1(-dt0)),
        "ssm_A_log": jnp.log(jax.random.uniform(next(ks), (N_B_LAYERS, 2, SSM_HEADS), F32, minval=1.0, maxval=16.0)),
        "ssm_D": 1.0 + nrm((N_B_LAYERS, 2, SSM_HEADS), 0.02),
        "ssm_norm": 1.0 + nrm((N_B_LAYERS, SSM_D_INNER), 0.02),
        "ssm_w_out": nrm((N_B_LAYERS, SSM_D_INNER, D), SSM_D_INNER ** -0.5),
        "na_wqkv": nrm((N_C_LAYERS, D, 3 * NA_HEADS * NA_HEAD_DIM), D ** -0.5),
        "na_wo": nrm((N_C_LAYERS, NA_HEADS * NA_HEAD_DIM, D), (NA_HEADS * NA_HEAD_DIM) ** -0.5),
        "na_rpb": nrm((N_C_LAYERS, NA_HEADS, 2 * NA_ROWS - 1, 2 * NA_COLS - 1), 0.02),
        "router_w": nrm((DEPTH, D, N_EXPERTS), D ** -0.5),
        "exp_w1": nrm((DEPTH, N_EXPERTS, D, EXPERT_FF), D ** -0.5),
        "exp_w3": nrm((DEPTH, N_EXPERTS, D, EXPERT_FF), D ** -0.5),
        "exp_w2": nrm((DEPTH, N_EXPERTS, EXPERT_FF, D), EXPERT_FF ** -0.5),
    }


def reference(x, c, ctx, c_ctx, ada_w, ada_b, norm_mix, norm_ffn, final_norm,
              da_wqkv, da_wo, da_lam_q1, da_lam_k1, da_lam_q2, da_lam_k2, da_subln,
              ssm_w_in, ssm_conv_w, ssm_conv_b, ssm_dt_bias, ssm_A_log, ssm_D, ssm_norm, ssm_w_out,
              na_wqkv, na_wo, na_rpb, router_w, exp_w1, exp_w3, exp_w2):
    cond_lat = jax.nn.silu(c)
    cond_ctx = jax.nn.silu(c_ctx)
    ia = ib = ic = 0
    for layer in range(DEPTH):
        need_ctx = layer < DEPTH - 1
        mod_l = (cond_lat @ ada_w[layer] + ada_b[layer])[:, None, :]
        mod_c = (cond_ctx @ ada_w[layer] + ada_b[layer])[None, None, :]
        sh_ml, sc_ml, g_ml, sh_fl, sc_fl, g_fl = jnp.split(mod_l, 6, axis=-1)
        sh_mc, sc_mc, g_mc, sh_fc, sc_fc, g_fc = jnp.split(mod_c, 6, axis=-1)
        h_l = modulate(rms_norm(x, norm_mix[layer]), sh_ml, sc_ml)
        h_c = modulate(rms_norm(ctx, norm_mix[layer]), sh_mc, sc_mc)
        kind = layer % N_MIXERS
        if kind == 0:
            o_c, o_l = diff_attention(h_c, h_l, da_wqkv[ia], da_wo[ia], da_lam_q1[ia], da_lam_k1[ia],
                                      da_lam_q2[ia], da_lam_k2[ia], da_subln[ia], lambda_init(layer), need_ctx)
            ia += 1
        elif kind == 1:
            o_c, o_l = mamba2_mixer(h_c, h_l, ssm_w_in[ib], ssm_conv_w[ib], ssm_conv_b[ib], ssm_dt_bias[ib],
                                    ssm_A_log[ib], ssm_D[ib], ssm_norm[ib], ssm_w_out[ib], need_ctx)
            ib += 1
        else:
            o_c, o_l = neighbourhood_attention(h_c, h_l, na_wqkv[ic], na_wo[ic], na_rpb[ic], need_ctx)
            ic += 1
        x = x + g_ml * o_l
        h_l = modulate(rms_norm(x, norm_ffn[layer]), sh_fl, sc_fl)
        x = x + g_fl * expert_choice_moe(h_l, router_w[layer], exp_w1[layer], exp_w3[layer], exp_w2[layer])
        if need_ctx:
            ctx = ctx + g_mc * o_c
            h_c = modulate(rms_norm(ctx, norm_ffn[layer]), sh_fc, sc_fc)
            ctx = ctx + g_fc * expert_choice_moe(h_c, router_w[layer], exp_w1[layer], exp_w3[layer], exp_w2[layer])
    return rms_norm(x, final_norm)
```

```python
import numpy as np
from contextlib import ExitStack
import concourse.bass as bass
import concourse.mybir as mybir
from concourse.bass_utils import run_bass_kernel_spmd

F32 = mybir.dt.float32
BF16 = mybir.dt.bfloat16
I32 = mybir.dt.int32
U32 = mybir.dt.uint32
AF = mybir.ActivationFunctionType
ALU = mybir.AluOpType
AX = mybir.AxisListType

ENGS = ["pe", "act", "dve", "pool", "sp"]
N_DMA_SEMS = 10


def _box(ap):
    t = ap.tensor
    shp = list(t.shape)
    dims = ap.ap
    off = int(ap.offset)
    if ap.space == "DRAM" or str(ap.space) == "DRAM":
        ext = sum((c - 1) * abs(s) for s, c in dims)
        return (0, 0, off, off + ext)
    F = 1
    for s in shp[1:]:
        F *= s
    if "PSum" in type(t).__name__:
        esz = mybir.dt.size(ap.dtype)
        f_lo0 = (off % F)
        f_hi0 = f_lo0 + sum((c - 1) * abs(s) for s, c in dims[1:])
        return (0, 127, (f_lo0 * esz) // 2048 * 2048, (f_hi0 * esz) // 2048 * 2048 + 2047)
    p_lo = off // F
    f_lo = off % F
    p_hi = p_lo + (dims[0][1] - 1) * (dims[0][0] // F if dims[0][0] else 0)
    f_hi = f_lo + sum((c - 1) * abs(s) for s, c in dims[1:])
    esz = mybir.dt.size(ap.dtype)
    return (p_lo, p_hi, f_lo * esz, (f_hi + 1) * esz - 1)


def _ovl(a, b):
    return not (a[1] < b[0] or b[1] < a[0] or a[3] < b[2] or b[3] < a[2])


def _covers(a, b):
    return a[0] <= b[0] and a[1] >= b[1] and a[2] <= b[2] and a[3] >= b[3]


class Prog:
    def __init__(self):
        self.nc = bass.Bass("TRN2", target_bir_lowering=False)
        self.es = ExitStack()
        self.ops = {e: [] for e in ENGS}
        self.cnt = {e: 0 for e in ENGS}
        self.seen = {e: {} for e in ENGS}
        self.track = {}
        self.ndma = 0
        self.dma_last = {}
        self.sems = {}
        self.out_tokens = []
        self.same_engine_sync = True
        for e in ["pe", "act", "dve", "pool"]:
            self.sems[e] = self.es.enter_context(self.nc.semaphore("s_" + e))
        self.qdma = {}
        for q in ("sp", "act", "pool"):
            self.qdma[q] = 0
            for i in range(N_DMA_SEMS):
                self.sems[("d", q, i)] = self.es.enter_context(self.nc.semaphore("s_d%s%d" % (q, i)))
        self.eng = {"pe": self.nc.tensor, "act": self.nc.scalar, "dve": self.nc.vector,
                    "pool": self.nc.gpsimd, "sp": self.nc.sync}
        self._uid = 0

    def dram(self, name, shape, dtype, kind):
        return self.nc.dram_tensor(name, list(shape), dtype, kind=kind).ap()

    def sb(self, name, shape, dtype=F32):
        return self.es.enter_context(self.nc.sbuf_tensor(name, list(shape), dtype))

    def ps(self, name, shape, dtype=F32):
        return self.es.enter_context(self.nc.psum_tensor(name, list(shape), dtype))

    def _deps(self, reads, writes, e=None):
        toks = []
        for ap in reads:
            nm = ap.tensor.name
            bx = _box(ap)
            isps = "PSum" in type(ap.tensor).__name__
            for ent in self.track.get(nm, []):
                if _ovl(ent[0], bx):
                    if ent[1] is not None:
                        toks.append(ent[1])
                    if isps:
                        toks.extend(t for t in ent[2] if t[0] != e)
        for ap in writes:
            nm = ap.tensor.name
            bx = _box(ap)
            for ent in self.track.get(nm, []):
                if _ovl(ent[0], bx):
                    if ent[1] is not None:
                        toks.append(ent[1])
                    toks.extend(ent[2])
        return toks

    def _commit(self, tok, reads, writes):
        for ap in reads:
            nm = ap.tensor.name
            bx = _box(ap)
            lst = self.track.setdefault(nm, [])
            hit = False
            for ent in lst:
                if _ovl(ent[0], bx):
                    if _covers(ent[0], bx) or True:
                        ent[2].append(tok)
                        hit = True
            if not hit:
                lst.append([bx, None, [tok]])
            else:
                if not any(_covers(ent[0], bx) for ent in lst):
                    lst.append([bx, None, [tok]])
        for ap in writes:
            nm = ap.tensor.name
            bx = _box(ap)
            lst = self.track.setdefault(nm, [])
            keep = []
            carry = []
            for ent in lst:
                if _covers(bx, ent[0]):
                    continue
                keep.append(ent)
            keep.append([bx, tok, []])
            self.track[nm] = keep
        for ap in reads:
            for ent in self.track.get(ap.tensor.name, []):
                if len(ent[2]) > 12:
                    best = {}
                    for k, v in ent[2]:
                        if best.get(k, -1) < v:
                            best[k] = v
                    ent[2] = list(best.items())

    def _waits(self, e, toks):
        best = {}
        for k, v in toks:
            if k == e and e == "pe":
                continue
            if k == e and not self.same_engine_sync:
                continue
            if best.get(k, -1) < v:
                best[k] = v
        out = []
        for k, v in best.items():
            if self.seen[e].get(k, -1) >= v:
                continue
            self.seen[e][k] = v
            out.append((k, v))
        return out

    def op(self, e, fn, reads=(), writes=(), pe_chain=False):
        reads = [r for r in reads if r is not None and not isinstance(r, (int, float))]
        writes = list(writes)
        toks = self._deps(reads, writes, e)
        waits = self._waits(e, toks)
        self.cnt[e] += 1
        tok = (e, self.cnt[e])
        self.ops[e].append((waits, fn, (e, 1)))
        self._commit(tok, reads, writes)
        return tok

    def dma(self, out, in_, q="sp", **kw):
        toks = self._deps([in_], [out])
        s = self.qdma[q] % N_DMA_SEMS
        k = self.qdma[q] // N_DMA_SEMS
        self.qdma[q] += 1
        self.ndma += 1
        key = ("d", q, s)
        if k > 0:
            toks.append((key, 16 * k))
        waits = self._waits(q, toks)
        tok = (key, 16 * (k + 1))
        self.ops[q].append((waits, lambda eng: eng.dma_start(out=out, in_=in_, **kw), (key, 16)))
        self._commit(tok, [in_], [out])
        if str(out.space) == "DRAM":
            self.out_tokens.append(tok)
        return tok

    def matmul(self, out, lhsT, rhs, start=True, stop=True):
        return self.op("pe", lambda eng: eng.matmul(out, lhsT, rhs, start=start, stop=stop),
                       reads=[lhsT, rhs] + ([] if start else [out]), writes=[out])

    def transpose(self, out, in_, ident):
        return self.op("pe", lambda eng: eng.transpose(out, in_, ident), reads=[in_, ident], writes=[out])

    def act(self, out, in_, func, bias=None, scale=None, accum_out=None, e="act"):
        kw = {}
        rd = [in_]
        if bias is not None:
            kw["bias"] = bias
            rd.append(bias)
        if scale is not None:
            kw["scale"] = scale
            rd.append(scale)
        wr = [out]
        if accum_out is not None:
            kw["accum_out"] = accum_out
            wr.append(accum_out)
        return self.op("act", lambda eng: eng.activation(out, in_, func, **kw), reads=rd, writes=wr)

    def tt(self, e, out, in0, in1, op):
        return self.op(e, lambda eng: eng.tensor_tensor(out, in0, in1, op), reads=[in0, in1], writes=[out])

    def ts(self, e, out, in0, s1, op0, s2=None, op1=None, accum_out=None):
        rd = [in0, s1, s2]
        wr = [out] + ([accum_out] if accum_out is not None else [])
        kw = {}
        if op1 is not None:
            kw["op1"] = op1
        if accum_out is not None:
            kw["accum_out"] = accum_out
        return self.op(e, lambda eng: eng.tensor_scalar(out, in0, s1, s2, op0, **kw), reads=rd, writes=wr)

    def stt(self, e, out, in0, scalar, in1, op0, op1):
        return self.op(e, lambda eng: eng.scalar_tensor_tensor(out, in0, scalar, in1, op0, op1),
                       reads=[in0, scalar, in1], writes=[out])

    def copy(self, e, out, in_):
        if e == "act":
            return self.op(e, lambda eng: eng.copy(out, in_), reads=[in_], writes=[out])
        return self.op(e, lambda eng: eng.tensor_copy(out, in_), reads=[in_], writes=[out])

    def memset(self, e, ap, val):
        return self.op(e, lambda eng: eng.memset(ap, val), reads=[], writes=[ap])

    def reduce(self, e, out, in_, op, axis=AX.X):
        return self.op(e, lambda eng: eng.tensor_reduce(out, in_, axis, op), reads=[in_], writes=[out])

    def recip(self, out, in_):
        return self.op("dve", lambda eng: eng.reciprocal(out, in_), reads=[in_], writes=[out])

    def finish(self):
        nc = self.nc
        fin = self._waits("sp", list(self.out_tokens))
        self.ops["sp"].append((fin, None, None))
        sems = self.sems
        ops = self.ops
        eng = self.eng

        def run(e, engine):
            for waits, fn, inc in ops[e]:
                for k, v in waits:
                    engine.wait_ge(sems[k], v)
                if fn is None:
                    continue
                ins = fn(engine)
                ins.then_inc(sems[inc[0]], inc[1])

        with nc.Block() as block:
            @block.tensor
            def _(t):
                run("pe", t)

            @block.scalar
            def _(t):
                run("act", t)

            @block.vector
            def _(t):
                run("dve", t)

            @block.gpsimd
            def _(t):
                run("pool", t)

            @block.sync
            def _(t):
                run("sp", t)
        self.es.close()
        return nc


EPS = 1e-6


class Ctx:
    pass


def load_w_bf16(P, w_dram, dst, K, N, stage, nstage=[0], cb=512):
    kc = K // 128
    wv = w_dram.rearrange("(c p) n -> p c n", p=128)
    engs = ["dve", "pool", "act"]
    for k0 in range(0, kc, 8):
        kn = min(8, kc - k0)
        for c0 in range(0, N, cb):
            cn = min(cb, N - c0)
            i = nstage[0]
            nstage[0] += 1
            st = stage[i % len(stage)]
            P.dma(st[:, :kn, :cn], wv[:, k0:k0 + kn, c0:c0 + cn], q="sp" if i % 2 == 0 else "act")
            P.copy(engs[i % 3], dst[:, k0:k0 + kn, c0:c0 + cn], st[:, :kn, :cn])


def rms_rstd(P, x, rows, D, junk, ss, tmp, rstd):
    P.act(junk[:rows, :D], x, AF.Square, accum_out=ss[:rows, :])
    P.ts("dve", tmp[:rows, :], ss[:rows, :], 1.0 / D, ALU.mult, EPS, ALU.add)
    P.act(tmp[:rows, :], tmp[:rows, :], AF.Sqrt)
    P.recip(rstd[:rows, :], tmp[:rows, :])


def norm_T(P, S, x, rows, wmod, sh, hT_dst, c0):
    rms_rstd(P, x, rows, 1024, S.junk, S.ss, S.tmp, S.rstd)
    P.ts("dve", S.xs[:rows, :], x, S.rstd[:rows, :], ALU.mult)
    for j in range(8):
        P.transpose(S.pT[:, j, :rows], S.xs[:rows, j * 128:(j + 1) * 128], S.ident[:rows, :rows])
    for j in range(8):
        if j < 4:
            P.act(hT_dst[:, j, c0:c0 + rows], S.pT[:, j, :rows], AF.Identity, bias=sh[:, j:j + 1], scale=wmod[:, j:j + 1])
        else:
            P.ts("dve", hT_dst[:, j, c0:c0 + rows], S.pT[:, j, :rows], wmod[:, j:j + 1], ALU.mult, sh[:, j:j + 1], ALU.add)


def mk_scratch(P, pfx=""):
    S = Ctx()
    S.junk = P.sb(pfx + "junk", [128, 1024])
    S.ss = P.sb(pfx + "ss", [128, 1])
    S.tmp = P.sb(pfx + "tmp", [128, 1])
    S.rstd = P.sb(pfx + "rstd", [128, 1])
    S.xs = P.sb(pfx + "xs", [128, 1024])
    S.pT = P.ps(pfx + "pT", [128, 8, 128])
    S.ident = P.sb(pfx + "ident_sb", [128, 128])
    return S


TILES = [(i * 128, 128) for i in range(16)] + [(2048, 64)]
BLOCKS = [(i * 512, 512) for i in range(4)] + [(2048, 64)]


def _idma(self, out, in_, idx_ap, bound):
    q = "pool"
    toks = self._deps([in_, idx_ap], [out], q)
    s = self.qdma[q] % N_DMA_SEMS
    k = self.qdma[q] // N_DMA_SEMS
    self.qdma[q] += 1
    key = ("d", q, s)
    if k > 0:
        toks.append((key, 16 * k))
    waits = self._waits(q, toks)
    tok = (key, 16 * (k + 1))
    self.ops[q].append((waits, lambda eng: eng.indirect_dma_start(
        out=out, out_offset=None, in_=in_, in_offset=bass.IndirectOffsetOnAxis(ap=idx_ap, axis=0),
        bounds_check=bound, oob_is_err=False), (key, 16)))
    self._commit(tok, [in_, idx_ap], [out])
    return tok


Prog.idma = _idma


def _barrier(self):
    toks = [(e, self.cnt[e]) for e in ("pe", "act", "dve", "pool") if self.cnt[e] > 0]
    for q in ("sp", "act", "pool"):
        nq = self.qdma[q]
        for s_ in range(min(nq, N_DMA_SEMS)):
            k = (nq - 1 - s_) // N_DMA_SEMS
            toks.append((("d", q, s_), 16 * (k + 1)))
    for e in ENGS:
        w = self._waits(e, list(toks))
        if w:
            self.ops[e].append((w, None, None))


class Arena:
    def __init__(self, P, name, nbytes):
        self.t = P.sb(name, [128, nbytes // 4], F32)
        self.tb = self.t.bitcast(BF16)
        self.nbytes = nbytes
        self.off = 0

    def reset(self):
        self.off = 0

    def alloc(self, shape, dtype=F32):
        esz = mybir.dt.size(dtype)
        n = 1
        for s_ in shape[1:]:
            n *= s_
        nb = (n * esz + 3) // 4 * 4
        assert self.off + nb <= self.nbytes, ("arena overflow", self.off, nb, self.nbytes)
        base = self.t if dtype == F32 else self.tb
        e0 = self.off // esz
        ap = base[0:shape[0], e0:e0 + n]
        self.off += nb
        if len(shape) == 3:
            ap = ap.rearrange("p (a b) -> p a b", b=shape[2])
        return ap


Prog.barrier = _barrier


import numpy as np
import ml_dtypes
BF = ml_dtypes.bfloat16
D = 1024

def fm(vec):
    return np.ascontiguousarray(vec.reshape(8, 128).T)

def core_rows(x, ctx, i):
    s, q = i // 4, i % 4
    return np.ascontiguousarray(np.concatenate([x[s, q * 2048:(q + 1) * 2048], ctx[s, q * 64:(q + 1) * 64]], 0))

def modpack(mods_l, norm_mix_l, norm_ffn_l, s):
    m = {}
    for vi, row in ((0, s), (1, 2)):
        parts = np.split(mods_l[row], 6)
        m[vi] = parts
    cols = [norm_mix_l, norm_ffn_l, m[0][0], m[1][0], m[0][1], m[1][1], m[0][3], m[1][3], m[0][4], m[1][4]]
    modp = np.ascontiguousarray(np.stack([fm(c) for c in cols], -1))
    bvecs = [m[0][2], m[1][2], m[0][5], m[1][5], norm_ffn_l, m[0][4], m[1][4], m[0][3], m[1][3]]
    bcp = np.ascontiguousarray(np.stack([np.broadcast_to(b, (128, 1024)) for b in bvecs], 0))
    return modp.astype(np.float32), bcp.astype(np.float32)

def rope_tables(i):
    q = i % 4
    t = np.arange(q * 2048, (q + 1) * 2048)
    rows = (t // 64).astype(np.float32); cols = (t % 64).astype(np.float32)
    freqs = (np.float32(10000.0) ** (-np.arange(16, dtype=np.float32) / 16)).astype(np.float32)
    ar = rows[:, None] * freqs[None]; ac = cols[:, None] * freqs[None]
    ang = np.concatenate([ar, ar, ac, ac], -1)
    cos = np.cos(ang).astype(np.float32); sin = np.sin(ang).astype(np.float32)
    cosT = np.ones((128, 2112), np.float32); sinT = np.zeros((128, 2112), np.float32)
    cosT[:, :2048] = np.concatenate([cos.T, cos.T], 0); sinT[:, :2048] = np.concatenate([sin.T, sin.T], 0)
    return cosT, sinT

def rope_RT():
    R = np.zeros((64, 64), np.float32)
    for d in range(64):
        seg = d // 16
        if seg % 2 == 0:
            R[d, d + 16] = -1.0
        else:
            R[d, d - 16] = 1.0
    R2 = np.zeros((128, 128), np.float32); R2[:64, :64] = R; R2[64:, 64:] = R
    return np.ascontiguousarray(R2.T)


import numpy as np

def moe_consts():
    BO = np.zeros((128, 128), np.float32); LT = np.zeros((128, 128), np.float32)
    for p in range(128):
        for p2 in range(128):
            if p // 32 == p2 // 32:
                BO[p, p2] = 1.0
                if p < p2:
                    LT[p, p2] = 1.0
    iota = np.ascontiguousarray(np.broadcast_to(np.arange(1024, dtype=np.float32), (128, 1024)))
    tokc = np.zeros((128, 2, 32, 2), np.float32)
    for fi in range(2):
        for pp in range(32):
            t = (2 * pp + fi) * 128 + np.arange(128)
            tokc[:, fi, pp, 0] = t // 64; tokc[:, fi, pp, 1] = t % 64
    tokcc = np.zeros((128, 2, 2), np.float32)
    for fi in range(2):
        t = fi * 128 + np.arange(128)
        tokcc[:, fi, 0] = t // 64; tokcc[:, fi, 1] = t % 64
    return {"ident": np.eye(128, dtype=np.float32), "BO": BO, "LT": LT, "iota": iota, "tokc": tokc, "tokcc": tokcc}

def l3_inputs(i, aff_lat, aff_ctx, hf_lat, hf_ctx, w1, w3, w2, consts):
    d = dict(consts)
    rowsA = []; rowsB = []
    for j in range(2):
        e = 2 * i + j
        for s in range(2):
            rowsA.append(aff_lat[s][:, e].reshape(32, 256))
            rowsB.append(aff_ctx[s][:, e])
    d["affA"] = np.ascontiguousarray(np.concatenate(rowsA, 0)); d["affB"] = np.ascontiguousarray(np.stack(rowsB, 0))
    for s in range(2):
        d["hf%d" % s] = hf_lat[s]; d["hfc%d" % s] = hf_ctx[s]
    for j in range(2):
        e = 2 * i + j
        d["w1_%d" % j] = w1[e]; d["w3_%d" % j] = w3[e]; d["w2_%d" % j] = w2[e]
    return d

def spos_token_major(sposA, sposB):
    lat = [np.zeros((8192, 16), np.int32) for _ in range(2)]
    ctx = [np.zeros((256, 16), np.int32) for _ in range(2)]
    for c in range(8):
        A = np.asarray(sposA[c]).reshape(4, 32 * 256); B = np.asarray(sposB[c])
        for j in range(2):
            for s in range(2):
                lat[s][:, 2 * c + j] = A[j * 2 + s]; ctx[s][:, 2 * c + j] = B[j * 2 + s]
    return lat, ctx

def l4_inputs(i, ye, yec, spos_lat, spos_ctx, x_mid_i, bcp, final_w=None):
    s, q = i // 4, i % 4
    ye_s = np.ascontiguousarray(np.stack([ye[e // 2][(e % 2) * 2 + s] for e in range(16)], 0))
    yec_s = np.ascontiguousarray(np.stack([yec[e // 2][(e % 2) * 2 + s] for e in range(16)], 0))
    sp = np.concatenate([spos_lat[s][q * 2048:(q + 1) * 2048], spos_ctx[s][q * 64:(q + 1) * 64]], 0)
    sposbc = np.ascontiguousarray(np.broadcast_to(sp.T[:, None, :], (16, 128, 2112)))
    slotid = (np.arange(8)[None, :, None] * 128 + np.arange(128)[:, None, None] + np.zeros((1, 1, 128))).astype(np.float32)
    d = {"ye_s": ye_s, "yec_s": yec_s, "sposbc": sposbc, "slotid": np.ascontiguousarray(slotid), "x_mid": x_mid_i,
         "gf": np.ascontiguousarray(bcp[2:4].transpose(1, 0, 2))}
    if final_w is not None:
        d["fnw"] = np.ascontiguousarray(np.broadcast_to(final_w, (128, 1024))).astype(np.float32)
    return d


import numpy as np

def na_bias_tables(rpb, qd):
    out = np.empty((3, 16, 768, 256), np.float32)
    for ti, b in enumerate((0, 3, 7)):
        r0 = 32 * qd + 4 * b
        qr = r0 + np.arange(4)[:, None]; qc = np.arange(64)[None, :]
        qr = np.broadcast_to(qr, (4, 64)).reshape(-1); qc = np.broadcast_to(qc, (4, 64)).reshape(-1)
        kr = (r0 - 4 + np.arange(12))[:, None]; kc = np.arange(64)[None, :]
        kr = np.broadcast_to(kr, (12, 64)).reshape(-1); kc = np.broadcast_to(kc, (12, 64)).reshape(-1)
        rs = np.clip(qr - 4, 0, 120); cs = np.clip(qc - 8, 0, 48)
        valid = ((kr[:, None] >= 0) & (kr[:, None] <= 127) & (kr[:, None] >= rs[None]) & (kr[:, None] < rs[None] + 8)
                 & (kc[:, None] >= cs[None]) & (kc[:, None] < cs[None] + 16))
        dr = np.clip(kr[:, None] - qr[None] + 7, 0, 14); dc = np.clip(kc[:, None] - qc[None] + 15, 0, 30)
        vals = rpb[:, dr, dc]
        out[ti] = np.where(valid[None], vals, np.float32(-30000.0))
    return np.ascontiguousarray(out.reshape(3, 16, 6, 128, 256).transpose(0, 1, 3, 2, 4))

def l2na_inputs(i, qT, kT, v, common):
    s, qd = i // 4, i % 4
    cores = list(range(4 * s, 4 * s + 4))
    kT_lat = np.concatenate([kT[c][:, :2048] for c in cores], 1)
    v_lat = np.concatenate([v[c][:2048] for c in cores], 0)
    kT_ctx = np.concatenate([kT[c][:, 2048:] for c in cores], 1)
    v_ctx = np.concatenate([v[c][2048:] for c in cores], 0)
    t0 = (32 * qd - 4) * 64
    kT_loc = np.zeros((1024, 2560), kT_lat.dtype); v_loc = np.zeros((2560, 1024), v_lat.dtype)
    lo = max(t0, 0); hi = min(t0 + 2560, 8192)
    kT_loc[:, lo - t0:hi - t0] = kT_lat[:, lo:hi]; v_loc[lo - t0:hi - t0] = v_lat[lo:hi]
    vh = np.ascontiguousarray(v_loc.reshape(20, 128, 16, 64).transpose(2, 1, 0, 3))
    vc = np.ascontiguousarray(v_ctx.reshape(2, 128, 16, 64).transpose(2, 1, 0, 3))
    d = dict(common)
    d.update({"qT": qT[i], "kT_loc": kT_loc, "kT_ctx": np.ascontiguousarray(kT_ctx), "vh": vh, "vc": vc})
    return d


import numpy as np

def ssd_consts():
    s = np.arange(128)[:, None]; l = np.arange(128)[None, :]
    tri = np.stack([(s <= l), (s >= l)], 0).astype(np.float32)
    neg = np.where(tri > 0, 0.0, -1e30).astype(np.float32)
    return {"ident": np.eye(128, dtype=np.float32), "tri": tri, "neg": neg}

def ssd_cols(g):
    z = np.arange(g * 512, (g + 1) * 512)
    xx = 2048 + np.arange(g * 512, (g + 1) * 512)
    B = 2048 + 2048 + np.arange(g * 128, (g + 1) * 128)
    Cc = 2048 + 2048 + 512 + np.arange(g * 128, (g + 1) * 128)
    dt = np.concatenate([5120 + d * 32 + np.arange(8 * g, 8 * g + 8) for d in range(2)])
    return z, xx, B, Cc, dt

def l1ssd_inputs(i, x_s, ctx_s, w_in, conv_w, conv_b, dt_bias, A_log, Dsk, modp, consts):
    g = i % 4
    z, xx, B, Cc, dt = ssd_cols(g)
    cols = np.concatenate([z, xx, B, Cc, dt])
    d = dict(consts)
    d["modp"] = modp
    d["x_all"] = np.ascontiguousarray(np.concatenate([ctx_s, x_s], 0))
    d["w_in_g"] = np.ascontiguousarray(w_in[:, cols])
    cch = np.concatenate([xx, B, Cc]) - 2048
    d["cw"] = np.ascontiguousarray(conv_w[:, cch].T.reshape(6, 128, 3).transpose(1, 0, 2))
    d["cb"] = np.ascontiguousarray(conv_b[cch].reshape(6, 128).T)
    hs = np.arange(8 * g, 8 * g + 8)
    bc = lambda a: np.ascontiguousarray(np.broadcast_to(np.concatenate([a[0, hs], a[1, hs]])[None, :], (128, 16))).astype(np.float32)
    d["dtb"] = bc(dt_bias); d["alog"] = bc(A_log); d["dsk"] = bc(Dsk)
    return d

def l2ssd_inputs(i, yz, common, ssm_norm, w_out):
    s, q = i // 4, i % 4
    full = np.concatenate([yz[4 * s + g] for g in range(4)], 1)
    rows = np.concatenate([full[256 + q * 2048:256 + (q + 1) * 2048], full[q * 64:(q + 1) * 64]], 0)
    d = dict(common)
    d["yz_rows"] = np.ascontiguousarray(rows)
    d["ssm_norm_bc"] = np.ascontiguousarray(np.broadcast_to(ssm_norm, (128, 2048))).astype(np.float32)
    d["w_out"] = w_out
    return d


def build_L0():
    P = Prog()
    condT = P.dram("condT", [128, 8, 3], F32, "ExternalInput")
    adaw = P.dram("ada_w", [1024, 6144], F32, "ExternalInput")
    adab = P.dram("ada_b3", [3, 6144], F32, "ExternalInput")
    out = P.dram("mods", [3, 6144], F32, "ExternalOutput")
    c_raw = P.sb("c_raw", [128, 8, 3]); c_s = P.sb("c_s", [128, 8, 3])
    b_sb = P.sb("b_sb", [3, 6144]); o_sb = P.sb("o_sb", [3, 6144])
    wbuf = [P.sb("wbuf%d" % i, [128, 8, 512]) for i in range(2)]
    ps = [P.ps("ps%d" % i, [3, 512]) for i in range(2)]
    P.dma(c_raw[:], condT[:, :, :])
    P.dma(b_sb[:], adab[:, :])
    P.act(c_s[:], c_raw[:], AF.Silu)
    awv = adaw.rearrange("(c p) n -> p c n", p=128)
    for cb in range(12):
        wb = wbuf[cb % 2]
        P.dma(wb[:], awv[:, :, cb * 512:(cb + 1) * 512], q="sp" if cb % 2 == 0 else "act")
        for k in range(8):
            P.matmul(ps[cb % 2][:], c_s[:, k, :], wb[:, k, :], start=(k == 0), stop=(k == 7))
        P.tt("dve", o_sb[:, cb * 512:(cb + 1) * 512], ps[cb % 2][:], b_sb[:, cb * 512:(cb + 1) * 512], ALU.add)
    P.dma(out[:, :], o_sb[:])
    return P.finish()


NT = 2112


def common_inputs(P, C):
    C.ident_d = P.dram("ident", [128, 128], F32, "ExternalInput")
    C.modp_d = P.dram("modp", [128, 8, 10], F32, "ExternalInput")
    C.modp = P.sb("modp_sb", [128, 8, 10])
    P.dma(C.modp[:], C.modp_d[:, :, :])


def wmod_sh(P, C, which):
    nw = 0 if which == "m" else 1
    shc = 2 if which == "m" else 6
    scc = 4 if which == "m" else 8
    wm, sh = [], []
    for v in range(2):
        t = P.sb("wmod_%s%d" % (which, v), [128, 8])
        P.ts("dve", t[:], C.modp[:, :, scc + v], 1.0, ALU.add)
        P.tt("dve", t[:], t[:], C.modp[:, :, nw], ALU.mult)
        wm.append(t)
        s = P.sb("shv_%s%d" % (which, v), [128, 8])
        P.copy("dve", s[:], C.modp[:, :, shc + v])
        sh.append(s)
    return wm, sh


def build_L1_DA(dbg=0):
    P = Prog()
    C = Ctx()
    common_inputs(P, C)
    x_d = P.dram("x_rows", [NT, 1024], F32, "ExternalInput")
    w_d = P.dram("wqkv", [1024, 3072], F32, "ExternalInput")
    cos_d = P.dram("cosT", [128, NT], F32, "ExternalInput")
    sin_d = P.dram("sinT", [128, NT], F32, "ExternalInput")
    RT_d = P.dram("RT", [128, 128], F32, "ExternalInput")
    qT_d = P.dram("qT", [1024, NT], BF16, "ExternalOutput")
    kT_d = P.dram("kT", [1024, NT], BF16, "ExternalOutput")
    v_d = P.dram("v", [NT, 1024], BF16, "ExternalOutput")

    S = mk_scratch(P)
    P.dma(S.ident[:], C.ident_d[:, :])
    cos = P.sb("cos", [128, NT]); sin = P.sb("sin", [128, NT])
    P.dma(cos[:], cos_d[:, :], q="act"); P.dma(sin[:], sin_d[:, :], q="act")
    RT32 = P.sb("RT32", [128, 128]); RT = P.sb("RTb", [128, 128], BF16)
    P.dma(RT32[:], RT_d[:, :]); P.copy("dve", RT[:], RT32[:])
    wm, sh = wmod_sh(P, C, "m")
    wsb = P.sb("wqkv_sb", [128, 8, 3072], BF16)
    stage = [P.sb("stg%d" % i, [128, 8, 512]) for i in range(2)]
    if dbg != 10:
        load_w_bf16(P, w_d, wsb, 1024, 3072, stage)
    xt = [P.sb("xt%d" % i, [128, 1024]) for i in range(2)]
    hT = [P.sb("hT%d" % i, [128, 8, 512], BF16) for i in range(2)]
    pq = [P.ps("pq%d" % i, [128, 512]) for i in range(2)]
    pr = [P.ps("pr%d" % i, [128, 512]) for i in range(2)]
    pv = [P.ps("pv%d" % i, [128, 512]) for i in range(2)]
    qb = [P.sb("qb%d" % i, [128, 512], BF16) for i in range(2)]
    t1 = [P.sb("t1_%d" % i, [128, 512]) for i in range(2)]
    t2 = [P.sb("t2_%d" % i, [128, 512]) for i in range(2)]
    qo = [P.sb("qo%d" % i, [128, 512], BF16) for i in range(3)]
    vo = [P.sb("vo%d" % i, [128, 1024], BF16) for i in range(2)]
    ti = 0
    n = 0
    for bi, (b0, bn) in enumerate(BLOCKS if dbg not in (10, 11) else []):
        if dbg == 12 and bn < 512:
            continue
        H = hT[bi % 2]
        v = 0 if b0 < 2048 else 1
        tiles = [(t0, tr) for (t0, tr) in TILES if b0 <= t0 < b0 + bn]
        if dbg >= 13:
            if bi > 0:
                continue
            tiles = tiles[:dbg - 12]
        for (t0, tr) in tiles:
            X = xt[ti % 2]; ti += 1
            P.dma(X[:tr, :], x_d[t0:t0 + tr, :], q="act")
            norm_T(P, S, X[:tr, :], tr, wm[v], sh[v], H, t0 - b0)
        for jc in (range(16) if dbg in (0, 3) else []):
            p = pq[n % 2]; r = pr[n % 2]
            for k in range(8):
                P.matmul(p[:, :bn], wsb[:, k, jc * 128:(jc + 1) * 128], H[:, k, :bn], start=(k == 0), stop=(k == 7))
            Q = qb[n % 2]
            P.copy("act", Q[:, :bn], p[:, :bn])
            P.matmul(r[:, :bn], RT[:], Q[:, :bn])
            P.tt("dve", t1[n % 2][:, :bn], Q[:, :bn], cos[:, b0:b0 + bn], ALU.mult)
            P.tt("dve", t2[n % 2][:, :bn], r[:, :bn], sin[:, b0:b0 + bn], ALU.mult)
            O = qo[n % 3]
            P.tt("pool", O[:, :bn], t1[n % 2][:, :bn], t2[n % 2][:, :bn], ALU.add)
            dst = qT_d if jc < 8 else kT_d
            jr = jc % 8
            P.dma(dst[jr * 128:(jr + 1) * 128, b0:b0 + bn], O[:, :bn], q="sp")
            n += 1
        for (t0, tr) in (tiles if dbg in (0, 2) else []):
            VO = vo[n % 2]
            for cb in range(2):
                p = pv[cb]
                for k in range(8):
                    P.matmul(p[:tr, :], H[:, k, t0 - b0:t0 - b0 + tr], wsb[:, k, 2048 + cb * 512:2048 + (cb + 1) * 512],
                             start=(k == 0), stop=(k == 7))
                P.copy("act" if cb == 0 else "dve", VO[:tr, cb * 512:(cb + 1) * 512], p[:tr, :])
            P.dma(v_d[t0:t0 + tr, :], VO[:tr, :], q="sp")
            n += 1
    return P.finish()


def post_mixer(P, C, S, o_src_fn, x_d, xmid_d, hf_d, aff_d, wo_sb, nK, bcp, wmf, shf, rw_sb, pw, pl):
    xt = [P.sb("pm_xt%d" % i, [128, 1024]) for i in range(2)]
    xm = [P.sb("pm_xm%d" % i, [128, 1024]) for i in range(2)]
    hf = [P.sb("pm_hf%d" % i, [128, 1024], BF16) for i in range(2)]
    hfT = P.sb("pm_hfT", [128, 8, 128])
    lg = P.sb("pm_lg", [128, 16]); mx = P.sb("pm_mx", [128, 1]); sm = P.sb("pm_sm", [128, 1])
    af = [P.sb("pm_af%d" % i, [128, 16]) for i in range(2)]
    for ti, (t0, tr) in enumerate(TILES):
        v = 0 if t0 < 2048 else 1
        X = xt[ti % 2]; XM = xm[ti % 2]; HF = hf[ti % 2]; AFF = af[ti % 2]
        P.dma(X[:tr, :], x_d[t0:t0 + tr, :], q="act")
        for cb in range(2):
            for k in range(nK):
                P.matmul(pw[cb][:tr, :], o_src_fn(k, t0, tr), wo_sb[:, k, cb * 512:(cb + 1) * 512], start=(k == 0), stop=(k == nK - 1))
            e = "dve" if cb == 0 else "pool"
            P.tt("dve", XM[:tr, cb * 512:(cb + 1) * 512], pw[cb][:tr, :], bcp[:, 0 + v, cb * 512:(cb + 1) * 512][:tr], ALU.mult)
            P.tt(e, XM[:tr, cb * 512:(cb + 1) * 512], XM[:tr, cb * 512:(cb + 1) * 512], X[:tr, cb * 512:(cb + 1) * 512], ALU.add)
        P.dma(xmid_d[t0:t0 + tr, :], XM[:tr, :], q="sp")
        norm_T(P, S, XM[:tr, :], tr, wmf[v], shf[v], hfT, 0)
        P.tt("pool", S.junk[:tr, :], S.xs[:tr, :], bcp[:, 4, :][:tr], ALU.mult)
        P.tt("pool", S.junk[:tr, :], S.junk[:tr, :], bcp[:, 5 + v, :][:tr], ALU.mult)
        P.tt("pool", HF[:tr, :], S.junk[:tr, :], bcp[:, 7 + v, :][:tr], ALU.add)
        P.dma(hf_d[t0:t0 + tr, :], HF[:tr, :], q="sp")
        for k in range(8):
            P.matmul(pl[:tr, :16], hfT[:, k, :tr], rw_sb[:, k, :], start=(k == 0), stop=(k == 7))
        P.reduce("dve", mx[:tr, :], pl[:tr, :16], ALU.max)
        P.ts("dve", mx[:tr, :], mx[:tr, :], -1.0, ALU.mult)
        P.act(lg[:tr, :], pl[:tr, :16], AF.Exp, bias=mx[:tr, :], accum_out=sm[:tr, :])
        P.recip(sm[:tr, :], sm[:tr, :])
        P.ts("dve", AFF[:tr, :], lg[:tr, :], sm[:tr, :], ALU.mult)
        P.dma(aff_d[t0:t0 + tr, :], AFF[:tr, :], q="sp")


def post_inputs(P, C):
    C.x_d = P.dram("x_rows", [NT, 1024], F32, "ExternalInput")
    C.bcp_d = P.dram("bcp", [128, 9, 1024], F32, "ExternalInput")
    C.rw_d = P.dram("router_w", [128, 8, 16], F32, "ExternalInput")
    C.xmid_d = P.dram("x_mid", [NT, 1024], F32, "ExternalOutput")
    C.hf_d = P.dram("h_f", [NT, 1024], BF16, "ExternalOutput")
    C.aff_d = P.dram("aff", [NT, 16], F32, "ExternalOutput")
    C.bcp = P.sb("bcp_sb", [128, 9, 1024])
    for i in range(9):
        P.dma(C.bcp[:, i, :], C.bcp_d[:, i, :], q="sp" if i % 2 else "act")
    for i in (5, 6):
        P.ts("pool", C.bcp[:, i, :], C.bcp[:, i, :], 1.0, ALU.add)
    C.rw = P.sb("rw_sb", [128, 8, 16])
    P.dma(C.rw[:], C.rw_d[:, :, :])


def build_L2_DA(dbg=0):
    P = Prog()
    C = Ctx()
    common_inputs(P, C)
    post_inputs(P, C)
    qT_d = P.dram("qT", [1024, NT], BF16, "ExternalInput")
    kT_d = P.dram("kT_full", [1024, 8448], BF16, "ExternalInput")
    vh_d = P.dram("vh", [8, 128, 66, 128], BF16, "ExternalInput")
    wo_d = P.dram("wo", [1024, 1024], F32, "ExternalInput")
    lam_d = P.dram("lamv", [128, 4, 64], F32, "ExternalInput")
    li_d = P.dram("laminit", [128, 2], F32, "ExternalInput")
    sub_d = P.dram("subln", [128, 1], F32, "ExternalInput")
    S = mk_scratch(P)
    P.dma(S.ident[:], C.ident_d[:, :])
    wmf, shf = wmod_sh(P, C, "f")
    lamv = P.sb("lamv_sb", [128, 4, 64]); li = P.sb("li_sb", [128, 2]); sub = P.sb("sub_sb", [128, 1])
    P.dma(lamv[:], lam_d[:, :, :]); P.dma(li[:], li_d[:, :]); P.dma(sub[:], sub_d[:, :])
    lt = P.sb("lam_t", [128, 2, 64]); ls = P.sb("lam_s", [128, 2]); nlam = P.sb("nlam", [128, 1]); subs = P.sb("subs", [128, 1])
    P.tt("dve", lt[:, 0, :], lamv[:, 0, :], lamv[:, 1, :], ALU.mult)
    P.tt("dve", lt[:, 1, :], lamv[:, 2, :], lamv[:, 3, :], ALU.mult)
    P.reduce("dve", ls[:, 0:1], lt[:, 0, :], ALU.add)
    P.reduce("dve", ls[:, 1:2], lt[:, 1, :], ALU.add)
    P.act(ls[:], ls[:], AF.Exp)
    P.tt("dve", nlam[:], ls[:, 1:2], ls[:, 0:1], ALU.subtract)
    P.tt("dve", nlam[:], nlam[:], li[:, 0:1], ALU.subtract)
    P.tt("dve", subs[:], sub[:], li[:, 1:2], ALU.mult)
    ones_b = P.sb("ones_b", [128, 128], BF16); ones_f = P.sb("ones_f", [128, 128])
    P.memset("dve", ones_b[:], 1.0); P.memset("dve", ones_f[:], 1.0)
    wo_sb = P.sb("wo_sb", [128, 8, 1024], BF16)
    stage = [P.sb("stg%d" % i, [128, 8, 256]) for i in range(2)]
    load_w_bf16(P, wo_d, wo_sb, 1024, 1024, stage, cb=256)
    onT = P.sb("onT", [128, 8, NT], BF16)
    if dbg:
        P.memset("pool", onT[:], 0.0)
    KT = [P.sb("KT%d" % i, [128, 8448], BF16) for i in range(1)]
    QT = [P.sb("QT%d" % i, [128, NT], BF16) for i in range(1)]
    V = [P.sb("V%d" % i, [128, 66, 128], BF16) for i in range(1)]
    ps = [P.ps("ps%d" % i, [128, 512]) for i in range(2)]
    po = [P.ps("po%d" % i, [128, 512]) for i in range(2)]
    pTf = S.pT[:].rearrange("p a b -> p (a b)")
    pd = [pTf[:, i * 512:(i + 1) * 512] for i in range(2)]
    px = P.ps("px", [128, 512])
    ET = [P.sb("ET%d" % i, [128, 512], BF16) for i in range(4)]
    r_ = [P.sb("r_%d" % i, [128, 512]) for i in range(2)]
    t_ = [P.sb("t_%d" % i, [128, 512]) for i in range(2)]
    o_ = P.sb("o_", [128, 512]); sq = P.sb("sq_", [128, 512]); rs = P.sb("rs_", [128, 512])
    n = 0
    heads = range(8) if dbg == 0 else range(dbg)
    for h in heads:
        K = KT[0]; Q = QT[0]; Vh = V[0]
        P.dma(K[:], kT_d[h * 128:(h + 1) * 128, :], q="sp")
        P.dma(Q[:], qT_d[h * 128:(h + 1) * 128, :], q="act")
        P.dma(Vh[:], vh_d[h, :, :, :], q="sp")
        for (b0, bn) in BLOCKS:
            kcs = range(66) if b0 < 2048 else range(2)
            nk = len(kcs)
            for m in range(2):
                for ki, kc in enumerate(kcs):
                    p = ps[n % 2]; E = ET[n % 4]; n += 1
                    P.matmul(p[:, :bn], K[m * 64:(m + 1) * 64, kc * 128:(kc + 1) * 128], Q[m * 64:(m + 1) * 64, b0:b0 + bn])
                    P.act(E[:, :bn], p[:, :bn], AF.Exp, scale=0.125)
                    P.matmul(po[m][:, :bn], Vh[:, kc, :], E[:, :bn], start=(ki == 0), stop=(ki == nk - 1))
                    P.matmul(pd[m][:, :bn], ones_b[:], E[:, :bn], start=(ki == 0), stop=(ki == nk - 1))
            for m in range(2):
                P.recip(r_[m][:, :bn], pd[m][:, :bn])
                P.tt("dve", t_[m][:, :bn], po[m][:, :bn], r_[m][:, :bn], ALU.mult)
            P.stt("dve", o_[:, :bn], t_[1][:, :bn], nlam[:, 0:1], t_[0][:, :bn], ALU.mult, ALU.add)
            P.act(sq[:, :bn], o_[:, :bn], AF.Square)
            P.matmul(px[:, :bn], ones_f[:], sq[:, :bn])
            P.ts("dve", rs[:, :bn], px[:, :bn], 1.0 / 128, ALU.mult, EPS, ALU.add)
            P.act(rs[:, :bn], rs[:, :bn], AF.Sqrt)
            P.recip(rs[:, :bn], rs[:, :bn])
            P.tt("dve", o_[:, :bn], o_[:, :bn], rs[:, :bn], ALU.mult)
            P.ts("pool", onT[:, h, b0:b0 + bn], o_[:, :bn], subs[:, 0:1], ALU.mult)
    post_mixer(P, C, S, lambda k, t0, tr: onT[:, k, t0:t0 + tr], C.x_d, C.xmid_d, C.hf_d, C.aff_d, wo_sb, 8, C.bcp,
               wmf, shf, C.rw, [ps[0], ps[1]], px)
    return P.finish()


CAP = 1024
CAPC = 32
NIT = 40


def build_L3(dbg=0):
    P = Prog()
    ident_d = P.dram("ident", [128, 128], F32, "ExternalInput")
    affA_d = P.dram("affA", [128, 256], F32, "ExternalInput")
    affB_d = P.dram("affB", [4, 256], F32, "ExternalInput")
    BO_d = P.dram("BO", [128, 128], F32, "ExternalInput")
    LT_d = P.dram("LT", [128, 128], F32, "ExternalInput")
    iota_d = P.dram("iota", [128, 1024], F32, "ExternalInput")
    tokc_d = P.dram("tokc", [128, 2, 32, 2], F32, "ExternalInput")
    tokcc_d = P.dram("tokcc", [128, 2, 2], F32, "ExternalInput")
    hf_d = [P.dram("hf%d" % s, [8192, 1024], BF16, "ExternalInput") for s in range(2)]
    hfc_d = [P.dram("hfc%d" % s, [256, 1024], BF16, "ExternalInput") for s in range(2)]
    w1_d = [P.dram("w1_%d" % j, [1024, 2048], F32, "ExternalInput") for j in range(2)]
    w3_d = [P.dram("w3_%d" % j, [1024, 2048], F32, "ExternalInput") for j in range(2)]
    w2_d = [P.dram("w2_%d" % j, [2048, 1024], F32, "ExternalInput") for j in range(2)]
    ye_d = P.dram("ye", [4, CAP, 1024], BF16, "ExternalOutput")
    yec_d = P.dram("yec", [4, CAPC, 1024], BF16, "ExternalOutput")
    sposA_d = P.dram("sposA", [128, 256], I32, "ExternalOutput")
    sposB_d = P.dram("sposB", [4, 256], I32, "ExternalOutput")

    ident = P.sb("ident_sb", [128, 128]); P.dma(ident[:], ident_d[:, :])
    identb = P.sb("identb", [128, 128], BF16); P.copy("dve", identb[:], ident[:])
    A = P.sb("A", [128, 256]); B = P.sb("B", [4, 256])
    BO = P.sb("BO_sb", [128, 128]); LT = P.sb("LT_sb", [128, 128]); iota = P.sb("iota_sb", [128, 1024])
    tokc = P.sb("tokc_sb", [128, 2, 32, 2]); tokcc = P.sb("tokcc_sb", [128, 2, 2])
    P.dma(A[:], affA_d[:, :]); P.dma(B[:], affB_d[:, :]); P.dma(BO[:], BO_d[:, :], q="act"); P.dma(LT[:], LT_d[:, :], q="act")
    P.dma(iota[:], iota_d[:, :]); P.dma(tokc[:], tokc_d[:, :, :, :], q="act"); P.dma(tokcc[:], tokcc_d[:, :, :], q="act")
    pcnt = P.ps("pcnt", [128, 512])
    junkA = P.sb("junkA", [128, 256]); junkB = P.sb("junkB", [4, 256])
    st = {}
    for nm, T, np_, K, junk in (("a", A, 128, CAP, junkA), ("b", B, 4, CAPC, junkB)):
        lo = P.sb("lo_" + nm, [np_, 1]); hi = P.sb("hi_" + nm, [np_, 1]); mid = P.sb("mid_" + nm, [np_, 1])
        cnt = P.sb("cnt_" + nm, [np_, 1]); ge = P.sb("ge_" + nm, [np_, 1]); d = P.sb("d_" + nm, [np_, 1])
        P.memset("dve", lo[:], 0.0); P.memset("dve", hi[:], 1.0)
        st[nm] = (T, np_, K, junk, lo, hi, mid, cnt, ge, d)
    for it in range(NIT):
        for nm in ("a", "b"):
            T, np_, K, junk, lo, hi, mid, cnt, ge, d = st[nm]
            P.ts("dve", mid[:], lo[:], hi[:, 0:1], ALU.add, 0.5, ALU.mult)
            P.ts("dve", junk[:], T[:], mid[:, 0:1], ALU.is_ge, 0.0, ALU.add, accum_out=cnt[:])
            if nm == "a":
                P.matmul(pcnt[:, 0:1], BO[:], cnt[:])
                P.ts("dve", ge[:], pcnt[:, 0:1], K - 0.5, ALU.is_ge)
            else:
                P.ts("dve", ge[:], cnt[:], K - 0.5, ALU.is_ge)
            P.tt("dve", d[:], mid[:], lo[:], ALU.subtract)
            P.stt("dve", lo[:], d[:], ge[:, 0:1], lo[:], ALU.mult, ALU.add)
            P.tt("dve", d[:], hi[:], mid[:], ALU.subtract)
            P.stt("dve", hi[:], d[:], ge[:, 0:1], mid[:], ALU.mult, ALU.add)
    onesA = P.sb("onesA", [128, 256]); P.memset("pool", onesA[:], 1.0)
    sp = {}
    for nm in ("a", "b"):
        T, np_, K, junk, lo, hi, mid, cnt, ge, d = st[nm]
        M = P.sb("M_" + nm, [np_, 256]); cum = P.sb("cum_" + nm, [np_, 256]); SP = P.sb("SP_" + nm, [np_, 256])
        SPi = P.sb("SPi_" + nm, [np_, 256], I32)
        P.ts("dve", M[:], T[:], lo[:, 0:1], ALU.is_ge)
        P.op("dve", lambda eng, cum=cum, M=M, np_=np_: eng.tensor_tensor_scan(cum[:], onesA[:np_, :], M[:], 0.0, ALU.mult, ALU.add),
             reads=[onesA[:np_, :], M[:]], writes=[cum[:]])
        if nm == "a":
            P.matmul(pcnt[:, 1:2], LT[:], cum[:, 255:256])
            P.ts("dve", SP[:], cum[:], pcnt[:, 1:2], ALU.add, -1.0 - K, ALU.add)
        else:
            P.ts("dve", SP[:], cum[:], -1.0 - K, ALU.add)
        P.tt("dve", SP[:], SP[:], M[:], ALU.mult)
        P.ts("dve", SP[:], SP[:], float(K), ALU.add)
        P.ts("dve", SP[:], SP[:], float(K), ALU.min)
        P.copy("dve", SPi[:], SP[:])
        sp[nm] = SP
        P.dma(sposA_d[:, :] if nm == "a" else sposB_d[:, :], SPi[:])
    pT = P.ps("pT", [128, 8, 128])
    TA = P.sb("TA", [128, 2, 2, 128])
    for qi, src in enumerate((sp["a"], A)):
        for fi in range(2):
            P.transpose(pT[:, qi * 2 + fi, :], src[:, fi * 128:(fi + 1) * 128], ident[:])
            P.copy("dve", TA[:, qi, fi, :], pT[:, qi * 2 + fi, :])
    TB = P.sb("TB", [128, 2, 2, 4])
    for qi, src in enumerate((sp["b"], B)):
        for fi in range(2):
            P.transpose(pT[:, 4 + qi * 2 + fi, 0:4], src[:, fi * 128:(fi + 1) * 128], ident[:4, :4])
            P.copy("dve", TB[:, qi, fi, :], pT[:, 4 + qi * 2 + fi, 0:4])
    R3 = P.sb("R3", [128, 2, 4, 32, 3])
    for fi in range(2):
        for r in range(4):
            P.copy("pool", R3[:, fi, r, :, 0:2], tokc[:, fi, :, :])
            P.copy("pool", R3[:, fi, r, :, 2], TA[:, 1, fi, r * 32:(r + 1) * 32])
    R3c = P.sb("R3c", [128, 2, 4, 3])
    for fi in range(2):
        for r in range(4):
            P.copy("pool", R3c[:, fi, r, 0:2], tokcc[:, fi, :])
            P.copy("pool", R3c[:, fi, r, 2:3], TB[:, 1, fi, r:r + 1])
    pidx = P.ps("pidx", [128, 512])
    pa = P.ps("pa", [128, 512]); pb = P.ps("pb", [128, 512])
    OH = [P.sb("OH%d" % i, [128, 1024]) for i in range(2)]
    RG = P.sb("RG", [3, 1024])
    n = 0
    for r in range(4):
        for tile in range(64):
            pp, fi = tile // 2, tile % 2
            O = OH[n % 2]; n += 1
            P.ts("dve" if n % 2 else "pool", O[:], iota[:], TA[:, 0, fi, r * 32 + pp:r * 32 + pp + 1], ALU.is_equal)
            P.matmul(pa[0:3, :], R3[:, fi, r, pp, :], O[:, 0:512], start=(tile == 0), stop=(tile == 63))
            P.matmul(pb[0:3, :], R3[:, fi, r, pp, :], O[:, 512:1024], start=(tile == 0), stop=(tile == 63))
        P.copy("dve", RG[:, 0:512], pa[0:3, :]); P.copy("act", RG[:, 512:1024], pb[0:3, :])
        for stl in range(8):
            c0 = (r * 8 + stl) * 3
            P.transpose(pidx[:, c0:c0 + 3], RG[0:3, stl * 128:(stl + 1) * 128], ident[0:3, 0:3])
    OHc = [P.sb("OHc%d" % i, [128, 32]) for i in range(2)]
    RGc = P.sb("RGc", [3, 32])
    for r in range(4):
        for tile in range(2):
            O = OHc[n % 2]; n += 1
            P.ts("dve", O[:], iota[:, 0:32], TB[:, 0, tile, r:r + 1], ALU.is_equal)
            P.matmul(pa[0:3, 0:32], R3c[:, tile, r, :], O[:], start=(tile == 0), stop=(tile == 1))
        P.copy("dve", RGc[:], pa[0:3, 0:32])
        P.transpose(pidx[:32, 128 + r * 3:128 + r * 3 + 3], RGc[0:3, :], ident[0:3, 0:3])
    IG = P.sb("IG", [128, 4, 8, 3]); IGc = P.sb("IGc", [32, 4, 3])
    P.copy("dve", IG[:].rearrange("p a b c -> p (a b c)"), pidx[:, 0:96])
    P.copy("dve", IGc[:].rearrange("p a c -> p (a c)"), pidx[:32, 128:140])
    idxf = P.sb("idxf", [128, 4, 8]); idxi = P.sb("idxi", [128, 4, 8], I32)
    P.ts("dve", idxf[:], IG[:, :, :, 0], 64.0, ALU.mult); P.tt("dve", idxf[:], idxf[:], IG[:, :, :, 1], ALU.add)
    P.copy("dve", idxi[:], idxf[:])
    idxcf = P.sb("idxcf", [32, 4]); idxci = P.sb("idxci", [32, 4], I32)
    P.ts("dve", idxcf[:], IGc[:, :, 0], 64.0, ALU.mult); P.tt("dve", idxcf[:], idxcf[:], IGc[:, :, 1], ALU.add)
    P.copy("dve", idxci[:], idxcf[:])
    if dbg == 1:
        dbg_d = P.dram("dbg_idx", [128, 32], I32, "ExternalOutput")
        P.dma(dbg_d[:, :], idxi[:].rearrange("p a b -> p (a b)"))
        dbg2_d = P.dram("dbg_gate", [128, 4, 8, 3], F32, "ExternalOutput")
        P.dma(dbg2_d[:, :, :, :], IG[:])
        return P.finish()
    W1 = P.sb("W1", [128, 8, 2048], BF16); W3 = P.sb("W3", [128, 8, 2048], BF16); W2 = P.sb("W2", [128, 16, 1024], BF16)
    stage = [P.sb("stg%d" % i, [128, 8, 512]) for i in range(2)]
    XS = [P.sb("XS%d" % i, [128, 1024], BF16) for i in range(4)]
    for t in XS:
        P.memset("pool", t[:], 0.0)
    xsT = P.sb("xsT", [128, 8, 512], BF16)
    h1T = P.sb("h1T", [128, 16, 512], BF16)
    pTb = P.ps("pTb", [128, 8, 128], BF16)
    pTf = pT[:].rearrange("p a b -> p (a b)"); py = [pTf[:, i * 512:(i + 1) * 512] for i in range(2)]
    sl = [P.sb("sl%d" % i, [128, 512]) for i in range(2)]
    yo = [P.sb("yo%d" % i, [128, 1024], BF16) for i in range(2)]
    nx = 0
    for j in range(2):
        load_w_bf16(P, w1_d[j], W1, 1024, 2048, stage)
        load_w_bf16(P, w3_d[j], W3, 1024, 2048, stage)
        load_w_bf16(P, w2_d[j], W2, 2048, 1024, stage)
        blocks = []
        for s in range(2):
            for b in range(2):
                blocks.append(("lat", s, b * 4, 4, 128))
        for s in range(2):
            blocks.append(("ctx", s, 0, 1, 32))
        for (kind, s, st0, nst, rows) in blocks:
            r = j * 2 + s
            bn = nst * rows
            for a in range(nst):
                X = XS[nx % 4]; nx += 1
                if kind == "lat":
                    P.idma(X[:rows, :], hf_d[s][:, :], idxi[:, r, st0 + a:st0 + a + 1], 8191)
                else:
                    P.idma(X[:rows, :], hfc_d[s][:, :], idxci[:, r:r + 1], 255)
                for k in range(8):
                    P.transpose(pTb[:, k, :rows], X[:rows, k * 128:(k + 1) * 128], identb[:rows, :rows])
                P.copy("dve", xsT[:, 0:4, a * rows:(a + 1) * rows], pTb[:, 0:4, :rows])
                P.copy("act", xsT[:, 4:8, a * rows:(a + 1) * rows], pTb[:, 4:8, :rows])
            for f in range(16):
                for k in range(8):
                    P.matmul(pa[:, :bn], W1[:, k, f * 128:(f + 1) * 128], xsT[:, k, :bn], start=(k == 0), stop=(k == 7))
                for k in range(8):
                    P.matmul(pb[:, :bn], W3[:, k, f * 128:(f + 1) * 128], xsT[:, k, :bn], start=(k == 0), stop=(k == 7))
                S_ = sl[f % 2]
                P.act(S_[:, :bn], pa[:, :bn], AF.Silu)
                P.tt("dve", h1T[:, f, :bn], S_[:, :bn], pb[:, :bn], ALU.mult)
            for a in range(nst):
                Y = yo[a % 2]
                for cb in range(2):
                    for f in range(16):
                        P.matmul(py[cb][:rows, :], h1T[:, f, a * rows:(a + 1) * rows], W2[:, f, cb * 512:(cb + 1) * 512], start=(f == 0), stop=(f == 15))
                    g = IG[:, r, st0 + a, 2:3] if kind == "lat" else IGc[:, r, 2:3]
                    if cb == 0:
                        P.ts("dve", Y[:rows, 0:512], py[cb][:rows, :], g[:rows], ALU.mult)
                    else:
                        P.act(Y[:rows, 512:1024], py[cb][:rows, :], AF.Copy, scale=g[:rows])
                if kind == "lat":
                    P.dma(ye_d[r, (st0 + a) * 128:(st0 + a + 1) * 128, :], Y[:rows, :], q="sp")
                else:
                    P.dma(yec_d[r, :, :], Y[:rows, :], q="sp")
    return P.finish()


def build_L4(final=False):
    NT = 2112
    P = Prog()
    ye_d = P.dram("ye_s", [16, CAP, 1024], BF16, "ExternalInput")
    yec_d = P.dram("yec_s", [16, CAPC, 1024], BF16, "ExternalInput")
    spos_d = P.dram("sposbc", [16, 128, NT], I32, "ExternalInput")
    slot_d = P.dram("slotid", [128, 8, 128], F32, "ExternalInput")
    xm_d = P.dram("x_mid", [NT, 1024], F32, "ExternalInput")
    gf_d = P.dram("gf", [128, 2, 1024], F32, "ExternalInput")
    out_d = P.dram("x_new", [NT, 1024], F32, "ExternalOutput")
    gf = P.sb("gf_sb", [128, 2, 1024]); P.dma(gf[:], gf_d[:, :, :])
    slot = P.sb("slot_sb", [128, 8, 128]); P.dma(slot[:], slot_d[:, :, :], q="act")
    if final:
        fn_d = P.dram("fnw", [128, 1024], F32, "ExternalInput")
        fnw = P.sb("fnw_sb", [128, 1024]); P.dma(fnw[:], fn_d[:, :], q="act")
        junk = P.sb("junk", [128, 1024]); ss = P.sb("ss", [128, 1]); tmp = P.sb("tmp", [128, 1]); rstd = P.sb("rstd", [128, 1])
    tiles = [(i * 128, 128) for i in range(16)] + [(2048, 64)]
    acc = [P.sb("acc%d" % i, [128, 1024]) for i in range(17)]
    YE = [P.sb("YE%d" % i, [128, 8, 1024], BF16) for i in range(2)]
    YEc = [P.sb("YEc%d" % i, [32, 1024], BF16) for i in range(2)]
    SPi = [P.sb("SPi%d" % i, [128, NT], I32) for i in range(2)]
    SPf = [P.sb("SPf%d" % i, [128, NT]) for i in range(2)]
    OHT = [P.sb("OHT%d" % i, [128, 8, 128], BF16) for i in range(3)]
    pc = [P.ps("pc%d" % i, [128, 512]) for i in range(4)]
    n = 0
    for e in range(16):
        Y = YE[e % 2]; Yc = YEc[e % 2]; SI = SPi[e % 2]; SF = SPf[e % 2]
        P.dma(Y[:], ye_d[e].rearrange("(c p) d -> p c d", p=128), q="sp")
        P.dma(Yc[:], yec_d[e, :, :], q="act")
        P.dma(SI[:], spos_d[e, :, :], q="act")
        P.copy("pool", SF[:], SI[:])
        for ti, (t0, tr) in enumerate(tiles):
            O = OHT[n % 3]
            lat = t0 < 2048
            if lat:
                in0 = SF[:, t0:t0 + 128].unsqueeze(1).to_broadcast([128, 8, 128])
                P.tt("dve", O[:], in0, slot[:], ALU.is_equal)
            else:
                P.tt("dve", O[:32, 0, :64], SF[:32, t0:t0 + 64], slot[:32, 0, :64], ALU.is_equal)
            for cb in range(2):
                p = pc[(n % 2) * 2 + cb]
                if lat:
                    for c in range(8):
                        P.matmul(p[:, :], O[:, c, :], Y[:, c, cb * 512:(cb + 1) * 512], start=(c == 0), stop=(c == 7))
                else:
                    P.matmul(p[:64, :], O[:32, 0, :64], Yc[:, cb * 512:(cb + 1) * 512])
                A = acc[ti]
                if e == 0:
                    P.copy("act", A[:tr, cb * 512:(cb + 1) * 512], p[:tr, :])
                else:
                    P.tt("dve", A[:tr, cb * 512:(cb + 1) * 512], A[:tr, cb * 512:(cb + 1) * 512], p[:tr, :], ALU.add)
            n += 1
    xm = [P.sb("xm%d" % i, [128, 1024]) for i in range(2)]
    for ti, (t0, tr) in enumerate(tiles):
        XM = xm[ti % 2]; AC = acc[ti]
        v = 0 if t0 < 2048 else 1
        P.dma(XM[:tr, :], xm_d[t0:t0 + tr, :], q="act")
        P.tt("dve", AC[:tr, :], AC[:tr, :], gf[:tr, v, :], ALU.mult)
        P.tt("pool", AC[:tr, :], AC[:tr, :], XM[:tr, :], ALU.add)
        if final:
            rms_rstd(P, AC[:tr, :], tr, 1024, junk, ss, tmp, rstd)
            P.ts("dve", AC[:tr, :], AC[:tr, :], rstd[:tr, :], ALU.mult)
            P.tt("dve", AC[:tr, :], AC[:tr, :], fnw[:tr, :], ALU.mult)
        P.dma(out_d[t0:t0 + tr, :], AC[:tr, :], q="sp")
    return P.finish()


NEG = -30000.0


def load_w_bf16_p(P, w_dram, dst, K, N, pp, stage, cb=256):
    kc = K // pp
    wv = w_dram.rearrange("(c p) n -> p c n", p=pp)
    i = 0
    for k0 in range(0, kc, 8):
        for c0 in range(0, N, cb):
            st = stage[i % 2]
            P.dma(st[:pp, :8, :cb], wv[:, k0:k0 + 8, c0:c0 + cb], q="sp" if i % 2 == 0 else "act")
            P.copy(["dve", "pool", "act"][i % 3], dst[:, k0:k0 + 8, c0:c0 + cb], st[:pp, :8, :cb])
            i += 1


def build_L2_NA():
    P = Prog()
    C = Ctx()
    common_inputs(P, C)
    post_inputs(P, C)
    qT_d = P.dram("qT", [1024, NT], BF16, "ExternalInput")
    kT_d = P.dram("kT_loc", [1024, 2560], BF16, "ExternalInput")
    kTc_d = P.dram("kT_ctx", [1024, 256], BF16, "ExternalInput")
    vh_d = P.dram("vh", [16, 128, 20, 64], BF16, "ExternalInput")
    vc_d = P.dram("vc", [16, 128, 2, 64], BF16, "ExternalInput")
    bias_d = P.dram("biasT", [3, 16, 128, 6, 256], F32, "ExternalInput")
    wo_d = P.dram("wo", [1024, 1024], F32, "ExternalInput")
    S = mk_scratch(P)
    P.dma(S.ident[:], C.ident_d[:, :])
    wmf, shf = wmod_sh(P, C, "f")
    ones_b = P.sb("ones_b", [128, 64], BF16); P.memset("dve", ones_b[:], 1.0)
    wo_sb = P.sb("wo_sb", [64, 16, 1024], BF16)
    stage = [P.sb("stg%d" % i, [128, 8, 128]) for i in range(2)]
    load_w_bf16_p(P, wo_d, wo_sb, 1024, 1024, 64, stage, cb=128)
    onT = P.sb("onT", [64, 16, NT], BF16)
    K = P.sb("K", [64, 2560], BF16); Kc = P.sb("Kc", [64, 256], BF16); Q = P.sb("Q", [64, NT], BF16)
    V = P.sb("V", [128, 20, 64], BF16); Vc = P.sb("Vc", [128, 2, 64], BF16)
    bias = [P.sb("bias%d" % i, [128, 6, 256]) for i in range(2)]
    ps = [P.ps("ps%d" % i, [128, 512]) for i in range(2)]
    po = P.ps("po", [128, 512]); pd = P.ps("pd", [128, 512]); px = P.ps("px", [128, 512])
    sb_ = [P.sb("sb_%d" % i, [128, 256]) for i in range(2)]
    ET = [P.sb("ET%d" % i, [128, 256], BF16) for i in range(4)]
    r_ = P.sb("r_", [64, 256])
    n = 0
    nb = 0
    for h in range(16):
        P.dma(K[:], kT_d[h * 64:(h + 1) * 64, :], q="sp"); P.dma(Kc[:], kTc_d[h * 64:(h + 1) * 64, :], q="sp")
        P.dma(Q[:], qT_d[h * 64:(h + 1) * 64, :], q="act")
        P.dma(V[:], vh_d[h, :, :, :], q="sp"); P.dma(Vc[:], vc_d[h, :, :, :], q="act")
        for b in range(9):
            if b < 8:
                q0, qn = b * 256, 256
                tb = 0 if b == 0 else (2 if b == 7 else 1)
                Bs = bias[nb % 2]; nb += 1
                P.dma(Bs[:], bias_d[tb, h, :, :, :], q="sp" if nb % 2 else "act")
                chunks = [("w", j) for j in range(6)] + [("c", j) for j in range(2)]
            else:
                q0, qn = 2048, 64
                chunks = [("c", j) for j in range(2)]
            for ci, (kind, j) in enumerate(chunks):
                p = ps[n % 2]; E = ET[n % 4]; Sb = sb_[n % 2]; n += 1
                if kind == "w":
                    lc = 2 * b + j
                    P.matmul(p[:, :qn], K[:, lc * 128:(lc + 1) * 128], Q[:, q0:q0 + qn])
                    P.stt("dve", Sb[:, :qn], p[:, :qn], 0.125, Bs[:, j, :qn], ALU.mult, ALU.add)
                    P.act(E[:, :qn], Sb[:, :qn], AF.Exp)
                    vv = V[:, lc, :]
                else:
                    P.matmul(p[:, :qn], Kc[:, j * 128:(j + 1) * 128], Q[:, q0:q0 + qn])
                    P.act(E[:, :qn], p[:, :qn], AF.Exp, scale=0.125)
                    vv = Vc[:, j, :]
                P.matmul(po[:64, :qn], vv, E[:, :qn], start=(ci == 0), stop=(ci == len(chunks) - 1))
                P.matmul(pd[:64, :qn], ones_b[:], E[:, :qn], start=(ci == 0), stop=(ci == len(chunks) - 1))
            P.recip(r_[:, :qn], pd[:64, :qn])
            P.tt("dve", onT[:, h, q0:q0 + qn], po[:64, :qn], r_[:, :qn], ALU.mult)
    post_mixer(P, C, S, lambda k, t0, tr: onT[:, k, t0:t0 + tr], C.x_d, C.xmid_d, C.hf_d, C.aff_d, wo_sb, 16, C.bcp,
               wmf, shf, C.rw, [ps[0], ps[1]], px)
    return P.finish()


NU = 8448
NCH = 66


def bc_mid(ap2, n):
    return ap2.unsqueeze(1).to_broadcast([ap2.shape[0], n, ap2.shape[1]])


def bc_last(ap2, n):
    return ap2.unsqueeze(2).to_broadcast([ap2.shape[0], ap2.shape[1], n])


def build_L1_SSD(dbg=0):
    P = Prog()
    C = Ctx()
    common_inputs(P, C)
    x_d = P.dram("x_all", [NU, 1024], F32, "ExternalInput")
    w_d = P.dram("w_in_g", [1024, 1296], F32, "ExternalInput")
    cw_d = P.dram("cw", [128, 6, 3], F32, "ExternalInput")
    cb_d = P.dram("cb", [128, 6], F32, "ExternalInput")
    dtb_d = P.dram("dtb", [128, 16], F32, "ExternalInput")
    alog_d = P.dram("alog", [128, 16], F32, "ExternalInput")
    dsk_d = P.dram("dsk", [128, 16], F32, "ExternalInput")
    tri_d = P.dram("tri", [2, 128, 128], F32, "ExternalInput")
    neg_d = P.dram("neg", [2, 128, 128], F32, "ExternalInput")
    yz_d = P.dram("yz", [NU, 512], BF16, "ExternalOutput")
    preT_d = P.dram("preT", [768, NU], F32, "Internal")
    zs_d = P.dram("zs", [NU, 512], BF16, "Internal")
    y1_d = P.dram("y1s", [NU, 512], F32, "Internal")

    S = mk_scratch(P)
    P.dma(S.ident[:], C.ident_d[:, :])
    identb = P.sb("identb", [128, 128], BF16); P.copy("dve", identb[:], S.ident[:])
    wm, sh = wmod_sh(P, C, "m")
    cw = P.sb("cw_sb", [128, 6, 3]); cb = P.sb("cb_sb", [128, 6]); dtb = P.sb("dtb_sb", [128, 16])
    A = P.sb("A_sb", [128, 16]); dsk = P.sb("dsk_sb", [128, 16]); Dsum = P.sb("Dsum", [128, 8])
    tri = P.sb("tri_sb", [128, 2, 128]); neg = P.sb("neg_sb", [128, 2, 128])
    P.dma(cw[:], cw_d[:, :, :]); P.dma(cb[:], cb_d[:, :]); P.dma(dtb[:], dtb_d[:, :]); P.dma(A[:], alog_d[:, :], q="act")
    P.dma(dsk[:], dsk_d[:, :], q="act")
    for d in range(2):
        P.dma(tri[:, d, :], tri_d[d, :, :], q="act"); P.dma(neg[:, d, :], neg_d[d, :, :], q="act")
    P.act(A[:], A[:], AF.Exp)
    P.ts("dve", A[:], A[:], -1.0, ALU.mult)
    P.tt("dve", Dsum[:], dsk[:, 0:8], dsk[:, 8:16], ALU.add)
    AR = Arena(P, "arena", 68 * 1024)
    wsb = AR.alloc([128, 8, 1280], BF16)
    wdt = AR.alloc([128, 8, 16])
    stage = [AR.alloc([128, 8, 128]) for i in range(2)]
    wv = w_d.rearrange("(c p) n -> p c n", p=128)
    for i in range(10):
        st = stage[i % 2]
        P.dma(st[:], wv[:, :, i * 128:(i + 1) * 128], q="sp" if i % 2 == 0 else "act")
        P.copy(["dve", "pool", "act"][i % 3], wsb[:, :, i * 128:(i + 1) * 128], st[:])
    P.dma(wdt[:], wv[:, :, 1280:1296])
    x_tm = P.sb("x_tm", [128, NCH, 512], BF16)
    BT = P.sb("BT", [128, NU], BF16); CT = P.sb("CT", [128, NU], BF16)
    B_tm = P.sb("B_tm", [128, NCH, 128], BF16)
    dt = P.sb("dt", [128, NCH, 16])
    pk = [P.ps("pk%d" % i, [128, 512]) for i in range(6)]
    xt = [AR.alloc([128, 1024]) for i in range(2)]
    hT32 = AR.alloc([128, 8, 512]); hTb = AR.alloc([128, 8, 512], BF16)
    ev = [AR.alloc([128, 512]) for i in range(2)]
    zo = [AR.alloc([128, 512], BF16) for i in range(2)]
    spa = P.sb("spa", [128, 16]); spe = P.sb("spe", [128, 16]); spx = P.sb("spx", [128, 16])
    n = 0
    for b0 in range(0, NU, 512):
        bn = min(512, NU - b0)
        nt = bn // 128
        for a in range(nt):
            u0 = b0 + a * 128
            v = 1 if u0 < 256 else 0
            X = xt[n % 2]; n += 1
            P.dma(X[:], x_d[u0:u0 + 128, :], q="act")
            norm_T(P, S, X[:], 128, wm[v], sh[v], hT32, a * 128)
        P.copy("pool", hTb[:, :, :bn], hT32[:, :, :bn])
        for j in range(6):
            p = pk[j % 2]
            for k in range(8):
                P.matmul(p[:, :bn], wsb[:, k, 512 + j * 128:512 + (j + 1) * 128], hTb[:, k, :bn], start=(k == 0), stop=(k == 7))
            E = ev[j % 2]
            P.copy("act" if j % 2 == 0 else "dve", E[:, :bn], p[:, :bn])
            P.dma(preT_d[j * 128:(j + 1) * 128, b0:b0 + bn], E[:, :bn], q="sp")
        for a in range(nt):
            u0 = b0 + a * 128
            p = pk[2 + a % 2]
            for k in range(8):
                P.matmul(p[:, :], hTb[:, k, a * 128:(a + 1) * 128], wsb[:, k, 0:512], start=(k == 0), stop=(k == 7))
            Z = zo[a % 2]
            P.act(Z[:], p[:], AF.Silu)
            P.dma(zs_d[u0:u0 + 128, :], Z[:], q="sp")
            pd_ = pk[4]
            for k in range(8):
                P.matmul(pd_[:, 0:16], hT32[:, k, a * 128:(a + 1) * 128], wdt[:, k, :], start=(k == 0), stop=(k == 7))
            P.tt("dve", spx[:], pd_[:, 0:16], dtb[:], ALU.add)
            P.ts("dve", spa[:], spx[:], -1.0, ALU.mult)
            P.tt("dve", spa[:], spa[:], spx[:], ALU.max)
            P.act(spe[:], spa[:], AF.Exp, scale=-1.0)
            P.act(spe[:], spe[:], AF.Ln, bias=1.0)
            P.ts("dve", spx[:], spx[:], 0.0, ALU.max)
            P.tt("dve", dt[:, u0 // 128, :], spx[:], spe[:], ALU.add)
    CW = 2048
    P.barrier(); AR.reset()
    pre = [AR.alloc([128, CW + 2]) for i in range(2)]
    cv = [AR.alloc([128, CW]) for i in range(2)]
    cvb = [AR.alloc([128, CW], BF16) for i in range(2)]
    pTb3 = pk[5].bitcast(BF16)[:, 0:1024].rearrange("p (a b) -> p a b", b=128)
    n = 0
    segs = [(0, 256)] + [(256 + i * CW, CW) for i in range(4)]
    for j in range(6):
        for (u0, un) in segs:
            lo_edge = (u0 == 0 or u0 == 256)
            hi_edge = (u0 + un == 256 or u0 + un == NU)
            Pr = pre[n % 2]; Cv = cv[n % 2]; Cb = cvb[n % 2]; n += 1
            if lo_edge:
                P.memset("pool", Pr[:, 0:1], 0.0)
            if hi_edge:
                P.memset("pool", Pr[:, un + 1:un + 2], 0.0)
            a0 = u0 - (0 if lo_edge else 1); a1 = u0 + un + (0 if hi_edge else 1)
            P.dma(Pr[:, (1 if lo_edge else 0):(1 if lo_edge else 0) + (a1 - a0)], preT_d[j * 128:(j + 1) * 128, a0:a1], q="act")
            P.ts("dve", Cv[:, :un], Pr[:, 1:un + 1], cw[:, j, 1:2], ALU.mult, cb[:, j:j + 1], ALU.add)
            P.stt("dve", Cv[:, :un], Pr[:, 0:un], cw[:, j, 0:1], Cv[:, :un], ALU.mult, ALU.add)
            P.stt("dve", Cv[:, :un], Pr[:, 2:un + 2], cw[:, j, 2:3], Cv[:, :un], ALU.mult, ALU.add)
            if j < 4 or j == 4:
                P.act(Cb[:, :un], Cv[:, :un], AF.Silu)
                if j == 4:
                    P.copy("pool", BT[:, u0:u0 + un], Cb[:, :un])
                for c in range(un // 128):
                    ch = (u0 // 128) + c
                    pt = pTb3[:, (c % 8), :]
                    P.transpose(pt, Cb[:, c * 128:(c + 1) * 128], identb[:])
                    dst = x_tm[:, ch, j * 128:(j + 1) * 128] if j < 4 else B_tm[:, ch, :]
                    P.copy("dve" if c % 2 else "act", dst, pt)
            else:
                P.act(CT[:, u0:u0 + un], Cv[:, :un], AF.Silu)
    if dbg == 1:
        d1 = P.dram("dbg_xtm", [128, NCH, 512], BF16, "ExternalOutput"); P.dma(d1[:, :, :], x_tm[:])
        d2 = P.dram("dbg_BT", [128, NU], BF16, "ExternalOutput"); P.dma(d2[:, :], BT[:])
        d3 = P.dram("dbg_CT", [128, NU], BF16, "ExternalOutput"); P.dma(d3[:, :], CT[:])
        d4 = P.dram("dbg_dt", [128, NCH, 16], F32, "ExternalOutput"); P.dma(d4[:, :, :], dt[:])
        d5 = P.dram("dbg_Btm", [128, NCH, 128], BF16, "ExternalOutput"); P.dma(d5[:, :, :], B_tm[:])
        return P.finish()
    pacs = S.pT[:].rearrange("p a b -> p (a b)")
    pcb, py, pyo, pst, psm = pk[0], pk[1], pk[2], pk[3], pk[4]
    P.barrier(); AR.reset()
    hst = AR.alloc([128, 512]); hbf = AR.alloc([128, 512], BF16)
    a_ = AR.alloc([128, 8]); abc = AR.alloc([128, 8, 128])
    acsrow = AR.alloc([128, 8, 128]); acscol = AR.alloc([128, 8])
    seg = AR.alloc([128, 8, 128]); MT = AR.alloc([128, 8, 128], BF16)
    eacs = AR.alloc([128, 8]); e2 = AR.alloc([128, 8]); w_ = AR.alloc([128, 8]); cdec = AR.alloc([128, 8])
    xdd = AR.alloc([128, 512], BF16)
    t_ = AR.alloc([128, 512]); yv = [AR.alloc([128, 512]) for i in range(2)]
    y1l = [AR.alloc([128, 512]) for i in range(2)]
    zl = [AR.alloc([128, 512], BF16) for i in range(2)]
    yo = [AR.alloc([128, 512], BF16) for i in range(2)]
    for d in (1, 0):
        order = list(range(NCH)) if d == 0 else [1, 0] + list(range(NCH - 1, 1, -1))
        lend = 127 if d == 0 else 0
        P.memset("dve", hst[:], 0.0); P.memset("dve", hbf[:], 0.0)
        for ci, c in enumerate(order):
            u0 = c * 128
            dtc = dt[:, c, d * 8:(d + 1) * 8]
            P.tt("dve", a_[:], dtc, A[:, d * 8:(d + 1) * 8], ALU.mult)
            P.copy("pool", abc[:], bc_last(a_[:], 128))
            for r in range(8):
                P.matmul(pacs[:, r * 128:(r + 1) * 128], abc[:, r, :], tri[:, d, :])
            P.matmul(psm[:, 0:8], tri[:, d, :], a_[:])
            P.copy("act", acsrow[:].rearrange("p a b -> p (a b)"), pacs)
            P.copy("dve", acscol[:], psm[:, 0:8])
            P.tt("dve", seg[:], acsrow[:], bc_last(acscol[:], 128), ALU.subtract)
            P.tt("pool", seg[:], seg[:], bc_mid(neg[:, d, :], 8), ALU.add)
            P.act(seg[:].rearrange("p a b -> p (a b)"), seg[:].rearrange("p a b -> p (a b)"), AF.Exp)
            P.matmul(pcb[:, 0:128], BT[:, u0:u0 + 128], CT[:, u0:u0 + 128])
            P.tt("dve", seg[:], seg[:], bc_mid(pcb[:, 0:128], 8), ALU.mult)
            P.tt("dve", MT[:], seg[:], bc_last(dtc, 128), ALU.mult)
            for r in range(8):
                P.matmul(py[:, r * 64:(r + 1) * 64], MT[:, r, :], x_tm[:, c, r * 64:(r + 1) * 64])
            P.matmul(pyo[:, :], CT[:, u0:u0 + 128], hbf[:])
            P.act(eacs[:], acscol[:], AF.Exp)
            P.tt("dve", t_[:].rearrange("p (a b) -> p a b", b=64), pyo[:].rearrange("p (a b) -> p a b", b=64), bc_last(eacs[:], 64), ALU.mult)
            Y = yv[ci % 2]
            P.tt("dve", Y[:], t_[:], py[:], ALU.add)
            P.tt("dve", e2[:], acsrow[:, :, lend], acscol[:], ALU.subtract)
            P.act(e2[:], e2[:], AF.Exp)
            P.tt("dve", w_[:], e2[:], dtc, ALU.mult)
            P.tt("pool", xdd[:].rearrange("p (a b) -> p a b", b=64), x_tm[:, c, :].rearrange("p (a b) -> p a b", b=64), bc_last(w_[:], 64), ALU.mult)
            P.matmul(pst[:, :], B_tm[:, c, :], xdd[:])
            P.act(cdec[:], acsrow[:, :, lend], AF.Exp)
            P.tt("dve", hst[:].rearrange("p (a b) -> p a b", b=64), hst[:].rearrange("p (a b) -> p a b", b=64), bc_last(cdec[:], 64), ALU.mult)
            P.tt("dve", hst[:], hst[:], pst[:], ALU.add)
            P.copy("act", hbf[:], hst[:])
            if d == 1:
                P.dma(y1_d[u0:u0 + 128, :], Y[:], q="sp")
            else:
                Y1 = y1l[ci % 2]; Z = zl[ci % 2]; O = yo[ci % 2]
                P.dma(Y1[:], y1_d[u0:u0 + 128, :], q="sp"); P.dma(Z[:], zs_d[u0:u0 + 128, :], q="sp")
                P.tt("pool", Y[:], Y[:], Y1[:], ALU.add)
                P.tt("dve", t_[:].rearrange("p (a b) -> p a b", b=64), x_tm[:, c, :].rearrange("p (a b) -> p a b", b=64), bc_last(Dsum[:], 64), ALU.mult)
                P.tt("dve", Y[:], Y[:], t_[:], ALU.add)
                P.tt("dve", O[:], Y[:], Z[:], ALU.mult)
                P.dma(yz_d[u0:u0 + 128, :], O[:], q="sp")
    return P.finish()


def build_L2_SSD():
    P = Prog()
    C = Ctx()
    common_inputs(P, C)
    post_inputs(P, C)
    yz_d = P.dram("yz_rows", [NT, 2048], BF16, "ExternalInput")
    nw_d = P.dram("ssm_norm_bc", [128, 2048], F32, "ExternalInput")
    wo_d = P.dram("w_out", [2048, 1024], F32, "ExternalInput")
    S = mk_scratch(P)
    P.dma(S.ident[:], C.ident_d[:, :])
    identb = P.sb("identb", [128, 128], BF16); P.copy("dve", identb[:], S.ident[:])
    wmf, shf = wmod_sh(P, C, "f")
    nw = P.sb("nw_sb", [128, 2048]); P.dma(nw[:], nw_d[:, :])
    wo_sb = P.sb("wo_sb", [128, 16, 1024], BF16)
    stage = [P.sb("stg%d" % i, [128, 8, 128]) for i in range(2)]
    load_w_bf16(P, wo_d, wo_sb, 2048, 1024, stage, cb=128)
    ynT = P.sb("ynT", [128, 16, NT], BF16)
    yzt = [P.sb("yzt%d" % i, [128, 2048], BF16) for i in range(2)]
    yf = P.sb("yf", [128, 2048]); ynb = P.sb("ynb", [128, 2048], BF16)
    ss = P.sb("g_ss", [128, 1]); tmp = P.sb("g_tmp", [128, 1]); rstd = P.sb("g_rstd", [128, 1])
    pa = P.ps("pa", [128, 512]); pb = P.ps("pb", [128, 512]); px = P.ps("px", [128, 512])
    pTb = [P.ps("pTb%d" % i, [128, 8, 128], BF16) for i in range(2)]
    for ti, (t0, tr) in enumerate(TILES):
        Y = yzt[ti % 2]
        P.dma(Y[:tr, :], yz_d[t0:t0 + tr, :], q="act")
        P.act(yf[:tr, :], Y[:tr, :], AF.Square, accum_out=ss[:tr, :])
        P.ts("dve", tmp[:tr, :], ss[:tr, :], 1.0 / 2048, ALU.mult, EPS, ALU.add)
        P.act(tmp[:tr, :], tmp[:tr, :], AF.Sqrt)
        P.recip(rstd[:tr, :], tmp[:tr, :])
        P.ts("dve", yf[:tr, :], Y[:tr, :], rstd[:tr, :], ALU.mult)
        P.tt("pool", ynb[:tr, :], yf[:tr, :], nw[:tr, :], ALU.mult)
        for half in range(2):
            pt = pTb[half]
            for k in range(8):
                kk = half * 8 + k
                P.transpose(pt[:, k, :tr], ynb[:tr, kk * 128:(kk + 1) * 128], identb[:tr, :tr])
            P.copy("act" if half == 0 else "dve", ynT[:, half * 8:(half + 1) * 8, t0:t0 + tr], pt[:, :, :tr])
    post_mixer(P, C, S, lambda k, t0, tr: ynT[:, k, t0:t0 + tr], C.x_d, C.xmid_d, C.hf_d, C.aff_d, wo_sb, 16, C.bcp,
               wmf, shf, C.rw, [pa, pb], px)
    return P.finish()


_PROGS = {}


def _prog(name, fn, *a):
    key = (name,) + a
    if key not in _PROGS:
        _PROGS[key] = fn(*a)
    return _PROGS[key]


def _run(nc, ins):
    res = run_bass_kernel_spmd(nc, ins, core_ids=list(range(8)))
    return res.results


def _lambda_init(layer):
    import math
    return 0.8 - 0.6 * math.exp(-0.3 * layer)


def kernel(**inp):
    inp = {k: np.asarray(v) for k, v in inp.items()}
    x = inp["x"]; ctx = inp["ctx"]
    eye = np.eye(128, dtype=np.float32)
    cond = np.stack([inp["c"][0], inp["c"][1], inp["c_ctx"]], 0)
    condT = np.ascontiguousarray(cond.reshape(3, 8, 128).transpose(2, 1, 0))
    r0 = _run(_prog("L0", build_L0), [{"condT": condT, "ada_w": inp["ada_w"][i % 4],
                                       "ada_b3": np.ascontiguousarray(np.broadcast_to(inp["ada_b"][i % 4], (3, 6144)))} for i in range(8)])
    mods = [r0[l]["mods"] for l in range(4)]
    mconsts = moe_consts()
    sconsts = ssd_consts()
    xs = [core_rows(x, ctx, i) for i in range(8)]
    ia = ib = ic = 0
    out = None
    for layer in range(4):
        kind = layer % 3
        mp = [modpack(mods[layer], inp["norm_mix"][layer], inp["norm_ffn"][layer], i // 4) for i in range(8)]
        rw = np.ascontiguousarray(inp["router_w"][layer].reshape(8, 128, 16).transpose(1, 0, 2))

        def common(i):
            return {"ident": eye, "modp": mp[i][0], "bcp": np.ascontiguousarray(mp[i][1].transpose(1, 0, 2)),
                    "x_rows": xs[i], "router_w": rw}
        if kind in (0, 2):
            wqkv = inp["da_wqkv"][ia] if kind == 0 else inp["na_wqkv"][ic]
            ins = []
            for i in range(8):
                if kind == 0:
                    cosT, sinT = rope_tables(i)
                else:
                    cosT, sinT = np.ones((128, 2112), np.float32), np.zeros((128, 2112), np.float32)
                ins.append({"ident": eye, "modp": mp[i][0], "x_rows": xs[i], "wqkv": wqkv, "cosT": cosT, "sinT": sinT, "RT": rope_RT()})
            r1 = _run(_prog("L1", build_L1_DA, 0), ins)
            qT = [r1[i]["qT"] for i in range(8)]; kT = [r1[i]["kT"] for i in range(8)]; v = [r1[i]["v"] for i in range(8)]
            if kind == 0:
                li = _lambda_init(layer)
                lamv = np.stack([inp["da_lam_q1"][ia], inp["da_lam_k1"][ia], inp["da_lam_q2"][ia], inp["da_lam_k2"][ia]], 0)
                ins = []
                for i in range(8):
                    s = i // 4
                    cores = range(4 * s, 4 * s + 4)
                    kT_full = np.concatenate([kT[c][:, 2048:] for c in cores] + [kT[c][:, :2048] for c in cores], 1)
                    v_full = np.concatenate([v[c][2048:] for c in cores] + [v[c][:2048] for c in cores], 0)
                    vh = np.ascontiguousarray(v_full.reshape(66, 128, 8, 128).transpose(2, 1, 0, 3))
                    d = common(i)
                    d.update({"qT": qT[i], "kT_full": np.ascontiguousarray(kT_full), "vh": vh, "wo": inp["da_wo"][ia],
                              "lamv": np.ascontiguousarray(np.broadcast_to(lamv, (128, 4, 64))).astype(np.float32),
                              "laminit": np.ascontiguousarray(np.broadcast_to(np.array([li, 1 - li], np.float32), (128, 2))),
                              "subln": np.ascontiguousarray(inp["da_subln"][ia].reshape(128, 1)).astype(np.float32)})
                    ins.append(d)
                r2 = _run(_prog("L2DA", build_L2_DA, 0), ins)
                ia += 1
            else:
                ins = []
                for i in range(8):
                    d = common(i)
                    d.update({"wo": inp["na_wo"][ic], "biasT": na_bias_tables(inp["na_rpb"][ic], i % 4)})
                    ins.append(l2na_inputs(i, qT, kT, v, d))
                r2 = _run(_prog("L2NA", build_L2_NA), ins)
                ic += 1
        else:
            xs_s = [np.concatenate([xs[4 * s + q][:2048] for q in range(4)], 0) for s in range(2)]
            cs_s = [np.concatenate([xs[4 * s + q][2048:] for q in range(4)], 0) for s in range(2)]
            ins = [l1ssd_inputs(i, xs_s[i // 4], cs_s[i // 4], inp["ssm_w_in"][ib], inp["ssm_conv_w"][ib], inp["ssm_conv_b"][ib],
                                inp["ssm_dt_bias"][ib], inp["ssm_A_log"][ib], inp["ssm_D"][ib], mp[i][0], sconsts) for i in range(8)]
            r1 = _run(_prog("L1S", build_L1_SSD, 0), ins)
            yz = [r1[i]["yz"] for i in range(8)]
            ins = [l2ssd_inputs(i, yz, common(i), inp["ssm_norm"][ib], inp["ssm_w_out"][ib]) for i in range(8)]
            r2 = _run(_prog("L2S", build_L2_SSD), ins)
            ib += 1
        aff_lat = [np.concatenate([r2[c]["aff"][:2048] for c in range(4 * s, 4 * s + 4)], 0) for s in range(2)]
        aff_ctx = [np.concatenate([r2[c]["aff"][2048:] for c in range(4 * s, 4 * s + 4)], 0) for s in range(2)]
        hf_lat = [np.ascontiguousarray(np.concatenate([r2[c]["h_f"][:2048] for c in range(4 * s, 4 * s + 4)], 0)) for s in range(2)]
        hf_ctx = [np.ascontiguousarray(np.concatenate([r2[c]["h_f"][2048:] for c in range(4 * s, 4 * s + 4)], 0)) for s in range(2)]
        ins = [l3_inputs(i, aff_lat, aff_ctx, hf_lat, hf_ctx, inp["exp_w1"][layer], inp["exp_w3"][layer], inp["exp_w2"][layer], mconsts)
               for i in range(8)]
        r3 = _run(_prog("L3", build_L3, 0), ins)
        ye = [r3[c]["ye"] for c in range(8)]; yec = [r3[c]["yec"] for c in range(8)]
        spl, spc = spos_token_major([r3[c]["sposA"] for c in range(8)], [r3[c]["sposB"] for c in range(8)])
        final = layer == 3
        ins = [l4_inputs(i, ye, yec, spl, spc, r2[i]["x_mid"], mp[i][1], inp["final_norm"] if final else None) for i in range(8)]
        r4 = _run(_prog("L4", build_L4, final), ins)
        xs = [r4[i]["x_new"] for i in range(8)]
    out = np.stack([np.concatenate([xs[4 * s + q][:2048] for q in range(4)], 0) for s in range(2)], 0)
    return np.ascontiguousarray(out.astype(np.float32, copy=False))
```

```python
import numpy as np
from contextlib import ExitStack
import concourse.bass as bass
import concourse.mybir as mybir
from concourse.bass_utils import run_bass_kernel_spmd

F32 = mybir.dt.float32
BF16 = mybir.dt.bfloat16
I32 = mybir.dt.int32
U32 = mybir.dt.uint32
AF = mybir.ActivationFunctionType
ALU = mybir.AluOpType
AX = mybir.AxisListType

ENGS = ["pe", "act", "dve", "pool", "sp"]
N_DMA_SEMS = 10


def _box(ap):
    t = ap.tensor
    shp = list(t.shape)
    dims = ap.ap
    off = int(ap.offset)
    if ap.space == "DRAM" or str(ap.space) == "DRAM":
        ext = sum((c - 1) * abs(s) for s, c in dims)
        return (0, 0, off, off + ext)
    F = 1
    for s in shp[1:]:
        F *= s
    if "PSum" in type(t).__name__:
        esz = mybir.dt.size(ap.dtype)
        f_lo0 = (off % F)
        f_hi0 = f_lo0 + sum((c - 1) * abs(s) for s, c in dims[1:])
        return (0, 127, (f_lo0 * esz) // 2048 * 2048, (f_hi0 * esz) // 2048 * 2048 + 2047)
    p_lo = off // F
    f_lo = off % F
    p_hi = p_lo + (dims[0][1] - 1) * (dims[0][0] // F if dims[0][0] else 0)
    f_hi = f_lo + sum((c - 1) * abs(s) for s, c in dims[1:])
    esz = mybir.dt.size(ap.dtype)
    return (p_lo, p_hi, f_lo * esz, (f_hi + 1) * esz - 1)


def _ovl(a, b):
    return not (a[1] < b[0] or b[1] < a[0] or a[3] < b[2] or b[3] < a[2])


def _covers(a, b):
    return a[0] <= b[0] and a[1] >= b[1] and a[2] <= b[2] and a[3] >= b[3]


class Prog:
    def __init__(self):
        self.nc = bass.Bass("TRN2", target_bir_lowering=False)
        self.es = ExitStack()
        self.ops = {e: [] for e in ENGS}
        self.cnt = {e: 0 for e in ENGS}
        self.seen = {e: {} for e in ENGS}
        self.track = {}
        self.ndma = 0
        self.dma_last = {}
        self.sems = {}
        self.out_tokens = []
        self.same_engine_sync = True
        for e in ["pe", "act", "dve", "pool"]:
            self.sems[e] = self.es.enter_context(self.nc.semaphore("s_" + e))
        self.qdma = {}
        for q in ("sp", "act", "pool"):
            self.qdma[q] = 0
            for i in range(N_DMA_SEMS):
                self.sems[("d", q, i)] = self.es.enter_context(self.nc.semaphore("s_d%s%d" % (q, i)))
        self.eng = {"pe": self.nc.tensor, "act": self.nc.scalar, "dve": self.nc.vector,
                    "pool": self.nc.gpsimd, "sp": self.nc.sync}
        self._uid = 0

    def dram(self, name, shape, dtype, kind):
        return self.nc.dram_tensor(name, list(shape), dtype, kind=kind).ap()

    def sb(self, name, shape, dtype=F32):
        return self.es.enter_context(self.nc.sbuf_tensor(name, list(shape), dtype))

    def ps(self, name, shape, dtype=F32):
        return self.es.enter_context(self.nc.psum_tensor(name, list(shape), dtype))

    def _deps(self, reads, writes, e=None):
        toks = []
        for ap in reads:
            nm = ap.tensor.name
            bx = _box(ap)
            isps = "PSum" in type(ap.tensor).__name__
            for ent in self.track.get(nm, []):
                if _ovl(ent[0], bx):
                    if ent[1] is not None:
                        toks.append(ent[1])
                    if isps:
                        toks.extend(t for t in ent[2] if t[0] != e)
        for ap in writes:
            nm = ap.tensor.name
            bx = _box(ap)
            for ent in self.track.get(nm, []):
                if _ovl(ent[0], bx):
                    if ent[1] is not None:
                        toks.append(ent[1])
                    toks.extend(ent[2])
        return toks

    def _commit(self, tok, reads, writes):
        for ap in reads:
            nm = ap.tensor.name
            bx = _box(ap)
            lst = self.track.setdefault(nm, [])
            hit = False
            for ent in lst:
                if _ovl(ent[0], bx):
                    if _covers(ent[0], bx) or True:
                        ent[2].append(tok)
                        hit = True
            if not hit:
                lst.append([bx, None, [tok]])
            else:
                if not any(_covers(ent[0], bx) for ent in lst):
                    lst.append([bx, None, [tok]])
        for ap in writes:
            nm = ap.tensor.name
            bx = _box(ap)
            lst = self.track.setdefault(nm, [])
            keep = []
            carry = []
            for ent in lst:
                if _covers(bx, ent[0]):
                    continue
                keep.append(ent)
            keep.append([bx, tok, []])
            self.track[nm] = keep
        for ap in reads:
            for ent in self.track.get(ap.tensor.name, []):
                if len(ent[2]) > 12:
                    best = {}
                    for k, v in ent[2]:
                        if best.get(k, -1) < v:
                            best[k] = v
                    ent[2] = list(best.items())

    def _waits(self, e, toks):
        best = {}
        for k, v in toks:
            if k == e and e == "pe":
                continue
            if k == e and not self.same_engine_sync:
                continue
            if best.get(k, -1) < v:
                best[k] = v
        out = []
        for k, v in best.items():
            if self.seen[e].get(k, -1) >= v:
                continue
            self.seen[e][k] = v
            out.append((k, v))
        return out

    def op(self, e, fn, reads=(), writes=(), pe_chain=False):
        reads = [r for r in reads if r is not None and not isinstance(r, (int, float))]
        writes = list(writes)
        toks = self._deps(reads, writes, e)
        waits = self._waits(e, toks)
        self.cnt[e] += 1
        tok = (e, self.cnt[e])
        self.ops[e].append((waits, fn, (e, 1)))
        self._commit(tok, reads, writes)
        return tok

    def dma(self, out, in_, q="sp", **kw):
        toks = self._deps([in_], [out])
        s = self.qdma[q] % N_DMA_SEMS
        k = self.qdma[q] // N_DMA_SEMS
        self.qdma[q] += 1
        self.ndma += 1
        key = ("d", q, s)
        if k > 0:
            toks.append((key, 16 * k))
        waits = self._waits(q, toks)
        tok = (key, 16 * (k + 1))
        self.ops[q].append((waits, lambda eng: eng.dma_start(out=out, in_=in_, **kw), (key, 16)))
        self._commit(tok, [in_], [out])
        if str(out.space) == "DRAM":
            self.out_tokens.append(tok)
        return tok

    def matmul(self, out, lhsT, rhs, start=True, stop=True):
        return self.op("pe", lambda eng: eng.matmul(out, lhsT, rhs, start=start, stop=stop),
                       reads=[lhsT, rhs] + ([] if start else [out]), writes=[out])

    def transpose(self, out, in_, ident):
        return self.op("pe", lambda eng: eng.transpose(out, in_, ident), reads=[in_, ident], writes=[out])

    def act(self, out, in_, func, bias=None, scale=None, accum_out=None, e="act"):
        kw = {}
        rd = [in_]
        if bias is not None:
            kw["bias"] = bias
            rd.append(bias)
        if scale is not None:
            kw["scale"] = scale
            rd.append(scale)
        wr = [out]
        if accum_out is not None:
            kw["accum_out"] = accum_out
            wr.append(accum_out)
        return self.op("act", lambda eng: eng.activation(out, in_, func, **kw), reads=rd, writes=wr)

    def tt(self, e, out, in0, in1, op):
        return self.op(e, lambda eng: eng.tensor_tensor(out, in0, in1, op), reads=[in0, in1], writes=[out])

    def ts(self, e, out, in0, s1, op0, s2=None, op1=None, accum_out=None):
        rd = [in0, s1, s2]
        wr = [out] + ([accum_out] if accum_out is not None else [])
        kw = {}
        if op1 is not None:
            kw["op1"] = op1
        if accum_out is not None:
            kw["accum_out"] = accum_out
        return self.op(e, lambda eng: eng.tensor_scalar(out, in0, s1, s2, op0, **kw), reads=rd, writes=wr)

    def stt(self, e, out, in0, scalar, in1, op0, op1):
        return self.op(e, lambda eng: eng.scalar_tensor_tensor(out, in0, scalar, in1, op0, op1),
                       reads=[in0, scalar, in1], writes=[out])

    def copy(self, e, out, in_):
        if e == "act":
            return self.op(e, lambda eng: eng.copy(out, in_), reads=[in_], writes=[out])
        return self.op(e, lambda eng: eng.tensor_copy(out, in_), reads=[in_], writes=[out])

    def memset(self, e, ap, val):
        return self.op(e, lambda eng: eng.memset(ap, val), reads=[], writes=[ap])

    def reduce(self, e, out, in_, op, axis=AX.X):
        return self.op(e, lambda eng: eng.tensor_reduce(out, in_, axis, op), reads=[in_], writes=[out])

    def recip(self, out, in_):
        return self.op("dve", lambda eng: eng.reciprocal(out, in_), reads=[in_], writes=[out])

    def finish(self):
        nc = self.nc
        fin = self._waits("sp", list(self.out_tokens))
        self.ops["sp"].append((fin, None, None))
        sems = self.sems
        ops = self.ops
        eng = self.eng

        def run(e, engine):
            for waits, fn, inc in ops[e]:
                for k, v in waits:
                    engine.wait_ge(sems[k], v)
                if fn is None:
                    continue
                ins = fn(engine)
                ins.then_inc(sems[inc[0]], inc[1])

        with nc.Block() as block:
            @block.tensor
            def _(t):
                run("pe", t)

            @block.scalar
            def _(t):
                run("act", t)

            @block.vector
            def _(t):
                run("dve", t)

            @block.gpsimd
            def _(t):
                run("pool", t)

            @block.sync
            def _(t):
                run("sp", t)
        self.es.close()
        return nc


EPS = 1e-6


class Ctx:
    pass


def load_w_bf16(P, w_dram, dst, K, N, stage, nstage=[0], cb=512):
    kc = K // 128
    wv = w_dram.rearrange("(c p) n -> p c n", p=128)
    engs = ["dve", "pool", "act"]
    for k0 in range(0, kc, 8):
        kn = min(8, kc - k0)
        for c0 in range(0, N, cb):
            cn = min(cb, N - c0)
            i = nstage[0]
            nstage[0] += 1
            st = stage[i % len(stage)]
            P.dma(st[:, :kn, :cn], wv[:, k0:k0 + kn, c0:c0 + cn], q="sp" if i % 2 == 0 else "act")
            P.copy(engs[i % 3], dst[:, k0:k0 + kn, c0:c0 + cn], st[:, :kn, :cn])


def rms_rstd(P, x, rows, D, junk, ss, tmp, rstd):
    P.act(junk[:rows, :D], x, AF.Square, accum_out=ss[:rows, :])
    P.ts("dve", tmp[:rows, :], ss[:rows, :], 1.0 / D, ALU.mult, EPS, ALU.add)
    P.act(tmp[:rows, :], tmp[:rows, :], AF.Sqrt)
    P.recip(rstd[:rows, :], tmp[:rows, :])


def norm_T(P, S, x, rows, wmod, sh, hT_dst, c0):
    rms_rstd(P, x, rows, 1024, S.junk, S.ss, S.tmp, S.rstd)
    P.ts("dve", S.xs[:rows, :], x, S.rstd[:rows, :], ALU.mult)
    for j in range(8):
        P.transpose(S.pT[:, j, :rows], S.xs[:rows, j * 128:(j + 1) * 128], S.ident[:rows, :rows])
    for j in range(8):
        if j < 4:
            P.act(hT_dst[:, j, c0:c0 + rows], S.pT[:, j, :rows], AF.Identity, bias=sh[:, j:j + 1], scale=wmod[:, j:j + 1])
        else:
            P.ts("dve", hT_dst[:, j, c0:c0 + rows], S.pT[:, j, :rows], wmod[:, j:j + 1], ALU.mult, sh[:, j:j + 1], ALU.add)


def mk_scratch(P, pfx=""):
    S = Ctx()
    S.junk = P.sb(pfx + "junk", [128, 1024])
    S.ss = P.sb(pfx + "ss", [128, 1])
    S.tmp = P.sb(pfx + "tmp", [128, 1])
    S.rstd = P.sb(pfx + "rstd", [128, 1])
    S.xs = P.sb(pfx + "xs", [128, 1024])
    S.pT = P.ps(pfx + "pT", [128, 8, 128])
    S.ident = P.sb(pfx + "ident_sb", [128, 128])
    return S


TILES = [(i * 128, 128) for i in range(16)] + [(2048, 64)]
BLOCKS = [(i * 512, 512) for i in range(4)] + [(2048, 64)]


def _idma(self, out, in_, idx_ap, bound):
    q = "pool"
    toks = self._deps([in_, idx_ap], [out], q)
    s = self.qdma[q] % N_DMA_SEMS
    k = self.qdma[q] // N_DMA_SEMS
    self.qdma[q] += 1
    key = ("d", q, s)
    if k > 0:
        toks.append((key, 16 * k))
    waits = self._waits(q, toks)
    tok = (key, 16 * (k + 1))
    self.ops[q].append((waits, lambda eng: eng.indirect_dma_start(
        out=out, out_offset=None, in_=in_, in_offset=bass.IndirectOffsetOnAxis(ap=idx_ap, axis=0),
        bounds_check=bound, oob_is_err=False), (key, 16)))
    self._commit(tok, [in_, idx_ap], [out])
    return tok


Prog.idma = _idma


def _barrier(self):
    toks = [(e, self.cnt[e]) for e in ("pe", "act", "dve", "pool") if self.cnt[e] > 0]
    for q in ("sp", "act", "pool"):
        nq = self.qdma[q]
        for s_ in range(min(nq, N_DMA_SEMS)):
            k = (nq - 1 - s_) // N_DMA_SEMS
            toks.append((("d", q, s_), 16 * (k + 1)))
    for e in ENGS:
        w = self._waits(e, list(toks))
        if w:
            self.ops[e].append((w, None, None))


class Arena:
    def __init__(self, P, name, nbytes):
        self.t = P.sb(name, [128, nbytes // 4], F32)
        self.tb = self.t.bitcast(BF16)
        self.nbytes = nbytes
        self.off = 0

    def reset(self):
        self.off = 0

    def alloc(self, shape, dtype=F32):
        esz = mybir.dt.size(dtype)
        n = 1
        for s_ in shape[1:]:
            n *= s_
        nb = (n * esz + 3) // 4 * 4
        assert self.off + nb <= self.nbytes, ("arena overflow", self.off, nb, self.nbytes)
        base = self.t if dtype == F32 else self.tb
        e0 = self.off // esz
        ap = base[0:shape[0], e0:e0 + n]
        self.off += nb
        if len(shape) == 3:
            ap = ap.rearrange("p (a b) -> p a b", b=shape[2])
        return ap


Prog.barrier = _barrier


import numpy as np
import ml_dtypes
BF = ml_dtypes.bfloat16
D = 1024

def fm(vec):
    return np.ascontiguousarray(vec.reshape(8, 128).T)

def core_rows(x, ctx, i):
    s, q = i // 4, i % 4
    return np.ascontiguousarray(np.concatenate([x[s, q * 2048:(q + 1) * 2048], ctx[s, q * 64:(q + 1) * 64]], 0))

def modpack(mods_l, norm_mix_l, norm_ffn_l, s):
    m = {}
    for vi, row in ((0, s), (1, 2)):
        parts = np.split(mods_l[row], 6)
        m[vi] = parts
    cols = [norm_mix_l, norm_ffn_l, m[0][0], m[1][0], m[0][1], m[1][1], m[0][3], m[1][3], m[0][4], m[1][4]]
    modp = np.ascontiguousarray(np.stack([fm(c) for c in cols], -1))
    bvecs = [m[0][2], m[1][2], m[0][5], m[1][5], norm_ffn_l, m[0][4], m[1][4], m[0][3], m[1][3]]
    bcp = np.ascontiguousarray(np.stack([np.broadcast_to(b, (128, 1024)) for b in bvecs], 0))
    return modp.astype(np.float32), bcp.astype(np.float32)

def rope_tables(i):
    q = i % 4
    t = np.arange(q * 2048, (q + 1) * 2048)
    rows = (t // 64).astype(np.float32); cols = (t % 64).astype(np.float32)
    freqs = (np.float32(10000.0) ** (-np.arange(16, dtype=np.float32) / 16)).astype(np.float32)
    ar = rows[:, None] * freqs[None]; ac = cols[:, None] * freqs[None]
    ang = np.concatenate([ar, ar, ac, ac], -1)
    cos = np.cos(ang).astype(np.float32); sin = np.sin(ang).astype(np.float32)
    cosT = np.ones((128, 2112), np.float32); sinT = np.zeros((128, 2112), np.float32)
    cosT[:, :2048] = np.concatenate([cos.T, cos.T], 0); sinT[:, :2048] = np.concatenate([sin.T, sin.T], 0)
    return cosT, sinT

def rope_RT():
    R = np.zeros((64, 64), np.float32)
    for d in range(64):
        seg = d // 16
        if seg % 2 == 0:
            R[d, d + 16] = -1.0
        else:
            R[d, d - 16] = 1.0
    R2 = np.zeros((128, 128), np.float32); R2[:64, :64] = R; R2[64:, 64:] = R
    return np.ascontiguousarray(R2.T)


import numpy as np

def moe_consts():
    BO = np.zeros((128, 128), np.float32); LT = np.zeros((128, 128), np.float32)
    for p in range(128):
        for p2 in range(128):
            if p // 32 == p2 // 32:
                BO[p, p2] = 1.0
                if p < p2:
                    LT[p, p2] = 1.0
    iota = np.ascontiguousarray(np.broadcast_to(np.arange(1024, dtype=np.float32), (128, 1024)))
    tokc = np.zeros((128, 2, 32, 2), np.float32)
    for fi in range(2):
        for pp in range(32):
            t = (2 * pp + fi) * 128 + np.arange(128)
            tokc[:, fi, pp, 0] = t // 64; tokc[:, fi, pp, 1] = t % 64
    tokcc = np.zeros((128, 2, 2), np.float32)
    for fi in range(2):
        t = fi * 128 + np.arange(128)
        tokcc[:, fi, 0] = t // 64; tokcc[:, fi, 1] = t % 64
    return {"ident": np.eye(128, dtype=np.float32), "BO": BO, "LT": LT, "iota": iota, "tokc": tokc, "tokcc": tokcc}

def l3_inputs(i, aff_lat, aff_ctx, hf_lat, hf_ctx, w1, w3, w2, consts):
    d = dict(consts)
    rowsA = []; rowsB = []
    for j in range(2):
        e = 2 * i + j
        for s in range(2):
            rowsA.append(aff_lat[s][:, e].reshape(32, 256))
            rowsB.append(aff_ctx[s][:, e])
    d["affA"] = np.ascontiguousarray(np.concatenate(rowsA, 0)); d["affB"] = np.ascontiguousarray(np.stack(rowsB, 0))
    for s in range(2):
        d["hf%d" % s] = hf_lat[s]; d["hfc%d" % s] = hf_ctx[s]
    for j in range(2):
        e = 2 * i + j
        d["w1_%d" % j] = w1[e]; d["w3_%d" % j] = w3[e]; d["w2_%d" % j] = w2[e]
    return d

def spos_token_major(sposA, sposB):
    lat = [np.zeros((8192, 16), np.int32) for _ in range(2)]
    ctx = [np.zeros((256, 16), np.int32) for _ in range(2)]
    for c in range(8):
        A = np.asarray(sposA[c]).reshape(4, 32 * 256); B = np.asarray(sposB[c])
        for j in range(2):
            for s in range(2):
                lat[s][:, 2 * c + j] = A[j * 2 + s]; ctx[s][:, 2 * c + j] = B[j * 2 + s]
    return lat, ctx

def l4_inputs(i, ye, yec, spos_lat, spos_ctx, x_mid_i, bcp, final_w=None):
    s, q = i // 4, i % 4
    ye_s = np.ascontiguousarray(np.stack([ye[e // 2][(e % 2) * 2 + s] for e in range(16)], 0))
    yec_s = np.ascontiguousarray(np.stack([yec[e // 2][(e % 2) * 2 + s] for e in range(16)], 0))
    sp = np.concatenate([spos_lat[s][q * 2048:(q + 1) * 2048], spos_ctx[s][q * 64:(q + 1) * 64]], 0)
    sposbc = np.ascontiguousarray(np.broadcast_to(sp.T[:, None, :], (16, 128, 2112)))
    slotid = (np.arange(8)[None, :, None] * 128 + np.arange(128)[:, None, None] + np.zeros((1, 1, 128))).astype(np.float32)
    d = {"ye_s": ye_s, "yec_s": yec_s, "sposbc": sposbc, "slotid": np.ascontiguousarray(slotid), "x_mid": x_mid_i,
         "gf": np.ascontiguousarray(bcp[2:4].transpose(1, 0, 2))}
    if final_w is not None:
        d["fnw"] = np.ascontiguousarray(np.broadcast_to(final_w, (128, 1024))).astype(np.float32)
    return d


import numpy as np

def na_bias_tables(rpb, qd):
    out = np.empty((3, 16, 768, 256), np.float32)
    for ti, b in enumerate((0, 3, 7)):
        r0 = 32 * qd + 4 * b
        qr = r0 + np.arange(4)[:, None]; qc = np.arange(64)[None, :]
        qr = np.broadcast_to(qr, (4, 64)).reshape(-1); qc = np.broadcast_to(qc, (4, 64)).reshape(-1)
        kr = (r0 - 4 + np.arange(12))[:, None]; kc = np.arange(64)[None, :]
        kr = np.broadcast_to(kr, (12, 64)).reshape(-1); kc = np.broadcast_to(kc, (12, 64)).reshape(-1)
        rs = np.clip(qr - 4, 0, 120); cs = np.clip(qc - 8, 0, 48)
        valid = ((kr[:, None] >= 0) & (kr[:, None] <= 127) & (kr[:, None] >= rs[None]) & (kr[:, None] < rs[None] + 8)
                 & (kc[:, None] >= cs[None]) & (kc[:, None] < cs[None] + 16))
        dr = np.clip(kr[:, None] - qr[None] + 7, 0, 14); dc = np.clip(kc[:, None] - qc[None] + 15, 0, 30)
        vals = rpb[:, dr, dc]
        out[ti] = np.where(valid[None], vals, np.float32(-30000.0))
    return np.ascontiguousarray(out.reshape(3, 16, 6, 128, 256).transpose(0, 1, 3, 2, 4))

def l2na_inputs(i, qT, kT, v, common):
    s, qd = i // 4, i % 4
    cores = list(range(4 * s, 4 * s + 4))
    kT_lat = np.concatenate([kT[c][:, :2048] for c in cores], 1)
    v_lat = np.concatenate([v[c][:2048] for c in cores], 0)
    kT_ctx = np.concatenate([kT[c][:, 2048:] for c in cores], 1)
    v_ctx = np.concatenate([v[c][2048:] for c in cores], 0)
    t0 = (32 * qd - 4) * 64
    kT_loc = np.zeros((1024, 2560), kT_lat.dtype); v_loc = np.zeros((2560, 1024), v_lat.dtype)
    lo = max(t0, 0); hi = min(t0 + 2560, 8192)
    kT_loc[:, lo - t0:hi - t0] = kT_lat[:, lo:hi]; v_loc[lo - t0:hi - t0] = v_lat[lo:hi]
    vh = np.ascontiguousarray(v_loc.reshape(20, 128, 16, 64).transpose(2, 1, 0, 3))
    vc = np.ascontiguousarray(v_ctx.reshape(2, 128, 16, 64).transpose(2, 1, 0, 3))
    d = dict(common)
    d.update({"qT": qT[i], "kT_loc": kT_loc, "kT_ctx": np.ascontiguousarray(kT_ctx), "vh": vh, "vc": vc})
    return d


import numpy as np

def ssd_consts():
    s = np.arange(128)[:, None]; l = np.arange(128)[None, :]
    tri = np.stack([(s <= l), (s >= l)], 0).astype(np.float32)
    neg = np.where(tri > 0, 0.0, -1e30).astype(np.float32)
    return {"ident": np.eye(128, dtype=np.float32), "tri": tri, "neg": neg}

def ssd_cols(g):
    z = np.arange(g * 512, (g + 1) * 512)
    xx = 2048 + np.arange(g * 512, (g + 1) * 512)
    B = 2048 + 2048 + np.arange(g * 128, (g + 1) * 128)
    Cc = 2048 + 2048 + 512 + np.arange(g * 128, (g + 1) * 128)
    dt = np.concatenate([5120 + d * 32 + np.arange(8 * g, 8 * g + 8) for d in range(2)])
    return z, xx, B, Cc, dt

def l1ssd_inputs(i, x_s, ctx_s, w_in, conv_w, conv_b, dt_bias, A_log, Dsk, modp, consts):
    g = i % 4
    z, xx, B, Cc, dt = ssd_cols(g)
    cols = np.concatenate([z, xx, B, Cc, dt])
    d = dict(consts)
    d["modp"] = modp
    d["x_all"] = np.ascontiguousarray(np.concatenate([ctx_s, x_s], 0))
    d["w_in_g"] = np.ascontiguousarray(w_in[:, cols])
    cch = np.concatenate([xx, B, Cc]) - 2048
    d["cw"] = np.ascontiguousarray(conv_w[:, cch].T.reshape(6, 128, 3).transpose(1, 0, 2))
    d["cb"] = np.ascontiguousarray(conv_b[cch].reshape(6, 128).T)
    hs = np.arange(8 * g, 8 * g + 8)
    bc = lambda a: np.ascontiguousarray(np.broadcast_to(np.concatenate([a[0, hs], a[1, hs]])[None, :], (128, 16))).astype(np.float32)
    d["dtb"] = bc(dt_bias); d["alog"] = bc(A_log); d["dsk"] = bc(Dsk)
    return d

def l2ssd_inputs(i, yz, common, ssm_norm, w_out):
    s, q = i // 4, i % 4
    full = np.concatenate([yz[4 * s + g] for g in range(4)], 1)
    rows = np.concatenate([full[256 + q * 2048:256 + (q + 1) * 2048], full[q * 64:(q + 1) * 64]], 0)
    d = dict(common)
    d["yz_rows"] = np.ascontiguousarray(rows)
    d["ssm_norm_bc"] = np.ascontiguousarray(np.broadcast_to(ssm_norm, (128, 2048))).astype(np.float32)
    d["w_out"] = w_out
    return d


def build_L0():
    P = Prog()
    condT = P.dram("condT", [128, 8, 3], F32, "ExternalInput")
    adaw = P.dram("ada_w", [1024, 6144], F32, "ExternalInput")
    adab = P.dram("ada_b3", [3, 6144], F32, "ExternalInput")
    out = P.dram("mods", [3, 6144], F32, "ExternalOutput")
    c_raw = P.sb("c_raw", [128, 8, 3]); c_s = P.sb("c_s", [128, 8, 3])
    b_sb = P.sb("b_sb", [3, 6144]); o_sb = P.sb("o_sb", [3, 6144])
    wbuf = [P.sb("wbuf%d" % i, [128, 8, 512]) for i in range(2)]
    ps = [P.ps("ps%d" % i, [3, 512]) for i in range(2)]
    P.dma(c_raw[:], condT[:, :, :])
    P.dma(b_sb[:], adab[:, :])
    P.act(c_s[:], c_raw[:], AF.Silu)
    awv = adaw.rearrange("(c p) n -> p c n", p=128)
    for cb in range(12):
        wb = wbuf[cb % 2]
        P.dma(wb[:], awv[:, :, cb * 512:(cb + 1) * 512], q="sp" if cb % 2 == 0 else "act")
        for k in range(8):
            P.matmul(ps[cb % 2][:], c_s[:, k, :], wb[:, k, :], start=(k == 0), stop=(k == 7))
        P.tt("dve", o_sb[:, cb * 512:(cb + 1) * 512], ps[cb % 2][:], b_sb[:, cb * 512:(cb + 1) * 512], ALU.add)
    P.dma(out[:, :], o_sb[:])
    return P.finish()


NT = 2112


def common_inputs(P, C):
    C.ident_d = P.dram("ident", [128, 128], F32, "ExternalInput")
    C.modp_d = P.dram("modp", [128, 8, 10], F32, "ExternalInput")
    C.modp = P.sb("modp_sb", [128, 8, 10])
    P.dma(C.modp[:], C.modp_d[:, :, :])


def wmod_sh(P, C, which):
    nw = 0 if which == "m" else 1
    shc = 2 if which == "m" else 6
    scc = 4 if which == "m" else 8
    wm, sh = [], []
    for v in range(2):
        t = P.sb("wmod_%s%d" % (which, v), [128, 8])
        P.ts("dve", t[:], C.modp[:, :, scc + v], 1.0, ALU.add)
        P.tt("dve", t[:], t[:], C.modp[:, :, nw], ALU.mult)
        wm.append(t)
        s = P.sb("shv_%s%d" % (which, v), [128, 8])
        P.copy("dve", s[:], C.modp[:, :, shc + v])
        sh.append(s)
    return wm, sh


def build_L1_DA(dbg=0):
    P = Prog()
    C = Ctx()
    common_inputs(P, C)
    x_d = P.dram("x_rows", [NT, 1024], F32, "ExternalInput")
    w_d = P.dram("wqkv", [1024, 3072], F32, "ExternalInput")
    cos_d = P.dram("cosT", [128, NT], F32, "ExternalInput")
    sin_d = P.dram("sinT", [128, NT], F32, "ExternalInput")
    RT_d = P.dram("RT", [128, 128], F32, "ExternalInput")
    qT_d = P.dram("qT", [1024, NT], BF16, "ExternalOutput")
    kT_d = P.dram("kT", [1024, NT], BF16, "ExternalOutput")
    v_d = P.dram("v", [NT, 1024], BF16, "ExternalOutput")

    S = mk_scratch(P)
    P.dma(S.ident[:], C.ident_d[:, :])
    cos = P.sb("cos", [128, NT]); sin = P.sb("sin", [128, NT])
    P.dma(cos[:], cos_d[:, :], q="act"); P.dma(sin[:], sin_d[:, :], q="act")
    RT32 = P.sb("RT32", [128, 128]); RT = P.sb("RTb", [128, 128], BF16)
    P.dma(RT32[:], RT_d[:, :]); P.copy("dve", RT[:], RT32[:])
    wm, sh = wmod_sh(P, C, "m")
    wsb = P.sb("wqkv_sb", [128, 8, 3072], BF16)
    stage = [P.sb("stg%d" % i, [128, 8, 512]) for i in range(2)]
    if dbg != 10:
        load_w_bf16(P, w_d, wsb, 1024, 3072, stage)
    xt = [P.sb("xt%d" % i, [128, 1024]) for i in range(2)]
    hT = [P.sb("hT%d" % i, [128, 8, 512], BF16) for i in range(2)]
    pq = [P.ps("pq%d" % i, [128, 512]) for i in range(2)]
    pr = [P.ps("pr%d" % i, [128, 512]) for i in range(2)]
    pv = [P.ps("pv%d" % i, [128, 512]) for i in range(2)]
    qb = [P.sb("qb%d" % i, [128, 512], BF16) for i in range(2)]
    t1 = [P.sb("t1_%d" % i, [128, 512]) for i in range(2)]
    t2 = [P.sb("t2_%d" % i, [128, 512]) for i in range(2)]
    qo = [P.sb("qo%d" % i, [128, 512], BF16) for i in range(3)]
    vo = [P.sb("vo%d" % i, [128, 1024], BF16) for i in range(2)]
    ti = 0
    n = 0
    for bi, (b0, bn) in enumerate(BLOCKS if dbg not in (10, 11) else []):
        if dbg == 12 and bn < 512:
            continue
        H = hT[bi % 2]
        v = 0 if b0 < 2048 else 1
        tiles = [(t0, tr) for (t0, tr) in TILES if b0 <= t0 < b0 + bn]
        if dbg >= 13:
            if bi > 0:
                continue
            tiles = tiles[:dbg - 12]
        for (t0, tr) in tiles:
            X = xt[ti % 2]; ti += 1
            P.dma(X[:tr, :], x_d[t0:t0 + tr, :], q="act")
            norm_T(P, S, X[:tr, :], tr, wm[v], sh[v], H, t0 - b0)
        for jc in (range(16) if dbg in (0, 3) else []):
            p = pq[n % 2]; r = pr[n % 2]
            for k in range(8):
                P.matmul(p[:, :bn], wsb[:, k, jc * 128:(jc + 1) * 128], H[:, k, :bn], start=(k == 0), stop=(k == 7))
            Q = qb[n % 2]
            P.copy("act", Q[:, :bn], p[:, :bn])
            P.matmul(r[:, :bn], RT[:], Q[:, :bn])
            P.tt("dve", t1[n % 2][:, :bn], Q[:, :bn], cos[:, b0:b0 + bn], ALU.mult)
            P.tt("dve", t2[n % 2][:, :bn], r[:, :bn], sin[:, b0:b0 + bn], ALU.mult)
            O = qo[n % 3]
            P.tt("pool", O[:, :bn], t1[n % 2][:, :bn], t2[n % 2][:, :bn], ALU.add)
            dst = qT_d if jc < 8 else kT_d
            jr = jc % 8
            P.dma(dst[jr * 128:(jr + 1) * 128, b0:b0 + bn], O[:, :bn], q="sp")
            n += 1
        for (t0, tr) in (tiles if dbg in (0, 2) else []):
            VO = vo[n % 2]
            for cb in range(2):
                p = pv[cb]
                for k in range(8):
                    P.matmul(p[:tr, :], H[:, k, t0 - b0:t0 - b0 + tr], wsb[:, k, 2048 + cb * 512:2048 + (cb + 1) * 512],
                             start=(k == 0), stop=(k == 7))
                P.copy("act" if cb == 0 else "dve", VO[:tr, cb * 512:(cb + 1) * 512], p[:tr, :])
            P.dma(v_d[t0:t0 + tr, :], VO[:tr, :], q="sp")
            n += 1
    return P.finish()


def post_mixer(P, C, S, o_src_fn, x_d, xmid_d, hf_d, aff_d, wo_sb, nK, bcp, wmf, shf, rw_sb, pw, pl):
    xt = [P.sb("pm_xt%d" % i, [128, 1024]) for i in range(2)]
    xm = [P.sb("pm_xm%d" % i, [128, 1024]) for i in range(2)]
    hf = [P.sb("pm_hf%d" % i, [128, 1024], BF16) for i in range(2)]
    hfT = P.sb("pm_hfT", [128, 8, 128])
    lg = P.sb("pm_lg", [128, 16]); mx = P.sb("pm_mx", [128, 1]); sm = P.sb("pm_sm", [128, 1])
    af = [P.sb("pm_af%d" % i, [128, 16]) for i in range(2)]
    for ti, (t0, tr) in enumerate(TILES):
        v = 0 if t0 < 2048 else 1
        X = xt[ti % 2]; XM = xm[ti % 2]; HF = hf[ti % 2]; AFF = af[ti % 2]
        P.dma(X[:tr, :], x_d[t0:t0 + tr, :], q="act")
        for cb in range(2):
            for k in range(nK):
                P.matmul(pw[cb][:tr, :], o_src_fn(k, t0, tr), wo_sb[:, k, cb * 512:(cb + 1) * 512], start=(k == 0), stop=(k == nK - 1))
            e = "dve" if cb == 0 else "pool"
            P.tt("dve", XM[:tr, cb * 512:(cb + 1) * 512], pw[cb][:tr, :], bcp[:, 0 + v, cb * 512:(cb + 1) * 512][:tr], ALU.mult)
            P.tt(e, XM[:tr, cb * 512:(cb + 1) * 512], XM[:tr, cb * 512:(cb + 1) * 512], X[:tr, cb * 512:(cb + 1) * 512], ALU.add)
        P.dma(xmid_d[t0:t0 + tr, :], XM[:tr, :], q="sp")
        norm_T(P, S, XM[:tr, :], tr, wmf[v], shf[v], hfT, 0)
        P.tt("pool", S.junk[:tr, :], S.xs[:tr, :], bcp[:, 4, :][:tr], ALU.mult)
        P.tt("pool", S.junk[:tr, :], S.junk[:tr, :], bcp[:, 5 + v, :][:tr], ALU.mult)
        P.tt("pool", HF[:tr, :], S.junk[:tr, :], bcp[:, 7 + v, :][:tr], ALU.add)
        P.dma(hf_d[t0:t0 + tr, :], HF[:tr, :], q="sp")
        for k in range(8):
            P.matmul(pl[:tr, :16], hfT[:, k, :tr], rw_sb[:, k, :], start=(k == 0), stop=(k == 7))
        P.reduce("dve", mx[:tr, :], pl[:tr, :16], ALU.max)
        P.ts("dve", mx[:tr, :], mx[:tr, :], -1.0, ALU.mult)
        P.act(lg[:tr, :], pl[:tr, :16], AF.Exp, bias=mx[:tr, :], accum_out=sm[:tr, :])
        P.recip(sm[:tr, :], sm[:tr, :])
        P.ts("dve", AFF[:tr, :], lg[:tr, :], sm[:tr, :], ALU.mult)
        P.dma(aff_d[t0:t0 + tr, :], AFF[:tr, :], q="sp")


def post_inputs(P, C):
    C.x_d = P.dram("x_rows", [NT, 1024], F32, "ExternalInput")
    C.bcp_d = P.dram("bcp", [128, 9, 1024], F32, "ExternalInput")
    C.rw_d = P.dram("router_w", [128, 8, 16], F32, "ExternalInput")
    C.xmid_d = P.dram("x_mid", [NT, 1024], F32, "ExternalOutput")
    C.hf_d = P.dram("h_f", [NT, 1024], BF16, "ExternalOutput")
    C.aff_d = P.dram("aff", [NT, 16], F32, "ExternalOutput")
    C.bcp = P.sb("bcp_sb", [128, 9, 1024])
    for i in range(9):
        P.dma(C.bcp[:, i, :], C.bcp_d[:, i, :], q="sp" if i % 2 else "act")
    for i in (5, 6):
        P.ts("pool", C.bcp[:, i, :], C.bcp[:, i, :], 1.0, ALU.add)
    C.rw = P.sb("rw_sb", [128, 8, 16])
    P.dma(C.rw[:], C.rw_d[:, :, :])


def build_L2_DA(dbg=0):
    P = Prog()
    C = Ctx()
    common_inputs(P, C)
    post_inputs(P, C)
    qT_d = P.dram("qT", [1024, NT], BF16, "ExternalInput")
    kT_d = P.dram("kT_full", [1024, 8448], BF16, "ExternalInput")
    vh_d = P.dram("vh", [8, 128, 66, 128], BF16, "ExternalInput")
    wo_d = P.dram("wo", [1024, 1024], F32, "ExternalInput")
    lam_d = P.dram("lamv", [128, 4, 64], F32, "ExternalInput")
    li_d = P.dram("laminit", [128, 2], F32, "ExternalInput")
    sub_d = P.dram("subln", [128, 1], F32, "ExternalInput")
    S = mk_scratch(P)
    P.dma(S.ident[:], C.ident_d[:, :])
    wmf, shf = wmod_sh(P, C, "f")
    lamv = P.sb("lamv_sb", [128, 4, 64]); li = P.sb("li_sb", [128, 2]); sub = P.sb("sub_sb", [128, 1])
    P.dma(lamv[:], lam_d[:, :, :]); P.dma(li[:], li_d[:, :]); P.dma(sub[:], sub_d[:, :])
    lt = P.sb("lam_t", [128, 2, 64]); ls = P.sb("lam_s", [128, 2]); nlam = P.sb("nlam", [128, 1]); subs = P.sb("subs", [128, 1])
    P.tt("dve", lt[:, 0, :], lamv[:, 0, :], lamv[:, 1, :], ALU.mult)
    P.tt("dve", lt[:, 1, :], lamv[:, 2, :], lamv[:, 3, :], ALU.mult)
    P.reduce("dve", ls[:, 0:1], lt[:, 0, :], ALU.add)
    P.reduce("dve", ls[:, 1:2], lt[:, 1, :], ALU.add)
    P.act(ls[:], ls[:], AF.Exp)
    P.tt("dve", nlam[:], ls[:, 1:2], ls[:, 0:1], ALU.subtract)
    P.tt("dve", nlam[:], nlam[:], li[:, 0:1], ALU.subtract)
    P.tt("dve", subs[:], sub[:], li[:, 1:2], ALU.mult)
    ones_b = P.sb("ones_b", [128, 128], BF16); ones_f = P.sb("ones_f", [128, 128])
    P.memset("dve", ones_b[:], 1.0); P.memset("dve", ones_f[:], 1.0)
    wo_sb = P.sb("wo_sb", [128, 8, 1024], BF16)
    stage = [P.sb("stg%d" % i, [128, 8, 256]) for i in range(2)]
    load_w_bf16(P, wo_d, wo_sb, 1024, 1024, stage, cb=256)
    onT = P.sb("onT", [128, 8, NT], BF16)
    if dbg:
        P.memset("pool", onT[:], 0.0)
    KT = [P.sb("KT%d" % i, [128, 8448], BF16) for i in range(1)]
    QT = [P.sb("QT%d" % i, [128, NT], BF16) for i in range(1)]
    V = [P.sb("V%d" % i, [128, 66, 128], BF16) for i in range(1)]
    ps = [P.ps("ps%d" % i, [128, 512]) for i in range(3)]
    po = [P.ps("po%d" % i, [128, 512]) for i in range(2)]
    pTf = S.pT[:].rearrange("p a b -> p (a b)")
    pd = [pTf[:, i * 512:(i + 1) * 512] for i in range(2)]
    px = P.ps("px", [128, 512])
    ET = [P.sb("ET%d" % i, [128, 512], BF16) for i in range(4)]
    r_ = [P.sb("r_%d" % i, [128, 512]) for i in range(2)]
    t_ = [P.sb("t_%d" % i, [128, 512]) for i in range(2)]
    o_ = P.sb("o_", [128, 512]); sq = P.sb("sq_", [128, 512]); rs = P.sb("rs_", [128, 512])
    dacc = [P.sb("dacc%d" % i, [128, 512]) for i in range(2)]
    n = 0
    heads = range(8) if dbg == 0 else range(dbg)
    for h in heads:
        K = KT[0]; Q = QT[0]; Vh = V[0]
        P.dma(K[:], kT_d[h * 128:(h + 1) * 128, :], q="sp")
        P.dma(Q[:], qT_d[h * 128:(h + 1) * 128, :], q="act")
        P.dma(Vh[:], vh_d[h, :, :, :], q="sp")
        for (b0, bn) in BLOCKS:
            kcs = range(66) if b0 < 2048 else range(2)
            nk = len(kcs)
            steps = [(m, ki, kc) for m in range(2) for ki, kc in enumerate(kcs)]
            LA = 2

            def emit_qk(j):
                m, ki, kc = steps[j]
                P.matmul(ps[(n + j) % 3][:, :bn], K[m * 64:(m + 1) * 64, kc * 128:(kc + 1) * 128], Q[m * 64:(m + 1) * 64, b0:b0 + bn])
            for j in range(min(LA, len(steps))):
                emit_qk(j)
            for j, (m, ki, kc) in enumerate(steps):
                if j + LA < len(steps):
                    emit_qk(j + LA)
                p = ps[(n + j) % 3]; E = ET[(n + j) % 4]
                P.act(E[:, :bn], p[:, :bn], AF.Exp, scale=0.125)
                P.matmul(po[m][:, :bn], Vh[:, kc, :], E[:, :bn], start=(ki == 0), stop=(ki == nk - 1))
                if ki == 0:
                    P.copy("dve", dacc[m][:, :bn], E[:, :bn])
                else:
                    P.tt("dve", dacc[m][:, :bn], dacc[m][:, :bn], E[:, :bn], ALU.add)
                if ki == nk - 1:
                    P.matmul(pd[m][:, :bn], ones_f[:], dacc[m][:, :bn])
            n += len(steps)
            for m in range(2):
                P.recip(r_[m][:, :bn], pd[m][:, :bn])
                P.tt("dve", t_[m][:, :bn], po[m][:, :bn], r_[m][:, :bn], ALU.mult)
            P.stt("dve", o_[:, :bn], t_[1][:, :bn], nlam[:, 0:1], t_[0][:, :bn], ALU.mult, ALU.add)
            P.act(sq[:, :bn], o_[:, :bn], AF.Square)
            P.matmul(px[:, :bn], ones_f[:], sq[:, :bn])
            P.ts("dve", rs[:, :bn], px[:, :bn], 1.0 / 128, ALU.mult, EPS, ALU.add)
            P.act(rs[:, :bn], rs[:, :bn], AF.Sqrt)
            P.recip(rs[:, :bn], rs[:, :bn])
            P.tt("dve", o_[:, :bn], o_[:, :bn], rs[:, :bn], ALU.mult)
            P.ts("pool", onT[:, h, b0:b0 + bn], o_[:, :bn], subs[:, 0:1], ALU.mult)
    post_mixer(P, C, S, lambda k, t0, tr: onT[:, k, t0:t0 + tr], C.x_d, C.xmid_d, C.hf_d, C.aff_d, wo_sb, 8, C.bcp,
               wmf, shf, C.rw, [ps[0], ps[1]], px)
    return P.finish()


CAP = 1024
CAPC = 32
NIT = 40


def build_L3(dbg=0):
    P = Prog()
    ident_d = P.dram("ident", [128, 128], F32, "ExternalInput")
    affA_d = P.dram("affA", [128, 256], F32, "ExternalInput")
    affB_d = P.dram("affB", [4, 256], F32, "ExternalInput")
    BO_d = P.dram("BO", [128, 128], F32, "ExternalInput")
    LT_d = P.dram("LT", [128, 128], F32, "ExternalInput")
    iota_d = P.dram("iota", [128, 1024], F32, "ExternalInput")
    tokc_d = P.dram("tokc", [128, 2, 32, 2], F32, "ExternalInput")
    tokcc_d = P.dram("tokcc", [128, 2, 2], F32, "ExternalInput")
    hf_d = [P.dram("hf%d" % s, [8192, 1024], BF16, "ExternalInput") for s in range(2)]
    hfc_d = [P.dram("hfc%d" % s, [256, 1024], BF16, "ExternalInput") for s in range(2)]
    w1_d = [P.dram("w1_%d" % j, [1024, 2048], F32, "ExternalInput") for j in range(2)]
    w3_d = [P.dram("w3_%d" % j, [1024, 2048], F32, "ExternalInput") for j in range(2)]
    w2_d = [P.dram("w2_%d" % j, [2048, 1024], F32, "ExternalInput") for j in range(2)]
    ye_d = P.dram("ye", [4, CAP, 1024], BF16, "ExternalOutput")
    yec_d = P.dram("yec", [4, CAPC, 1024], BF16, "ExternalOutput")
    sposA_d = P.dram("sposA", [128, 256], I32, "ExternalOutput")
    sposB_d = P.dram("sposB", [4, 256], I32, "ExternalOutput")

    ident = P.sb("ident_sb", [128, 128]); P.dma(ident[:], ident_d[:, :])
    identb = P.sb("identb", [128, 128], BF16); P.copy("dve", identb[:], ident[:])
    A = P.sb("A", [128, 256]); B = P.sb("B", [4, 256])
    BO = P.sb("BO_sb", [128, 128]); LT = P.sb("LT_sb", [128, 128]); iota = P.sb("iota_sb", [128, 1024])
    tokc = P.sb("tokc_sb", [128, 2, 32, 2]); tokcc = P.sb("tokcc_sb", [128, 2, 2])
    P.dma(A[:], affA_d[:, :]); P.dma(B[:], affB_d[:, :]); P.dma(BO[:], BO_d[:, :], q="act"); P.dma(LT[:], LT_d[:, :], q="act")
    P.dma(iota[:], iota_d[:, :]); P.dma(tokc[:], tokc_d[:, :, :, :], q="act"); P.dma(tokcc[:], tokcc_d[:, :, :], q="act")
    pcnt = P.ps("pcnt", [128, 512])
    junkA = P.sb("junkA", [128, 256]); junkB = P.sb("junkB", [4, 256])
    st = {}
    for nm, T, np_, K, junk in (("a", A, 128, CAP, junkA), ("b", B, 4, CAPC, junkB)):
        lo = P.sb("lo_" + nm, [np_, 1]); hi = P.sb("hi_" + nm, [np_, 1]); mid = P.sb("mid_" + nm, [np_, 1])
        cnt = P.sb("cnt_" + nm, [np_, 1]); ge = P.sb("ge_" + nm, [np_, 1]); d = P.sb("d_" + nm, [np_, 1])
        P.memset("dve", lo[:], 0.0); P.memset("dve", hi[:], 1.0)
        st[nm] = (T, np_, K, junk, lo, hi, mid, cnt, ge, d)
    for it in range(NIT):
        for nm in ("a", "b"):
            T, np_, K, junk, lo, hi, mid, cnt, ge, d = st[nm]
            P.ts("dve", mid[:], lo[:], hi[:, 0:1], ALU.add, 0.5, ALU.mult)
            P.ts("dve", junk[:], T[:], mid[:, 0:1], ALU.is_ge, 0.0, ALU.add, accum_out=cnt[:])
            if nm == "a":
                P.matmul(pcnt[:, 0:1], BO[:], cnt[:])
                P.ts("dve", ge[:], pcnt[:, 0:1], K - 0.5, ALU.is_ge)
            else:
                P.ts("dve", ge[:], cnt[:], K - 0.5, ALU.is_ge)
            P.tt("dve", d[:], mid[:], lo[:], ALU.subtract)
            P.stt("dve", lo[:], d[:], ge[:, 0:1], lo[:], ALU.mult, ALU.add)
            P.tt("dve", d[:], hi[:], mid[:], ALU.subtract)
            P.stt("dve", hi[:], d[:], ge[:, 0:1], mid[:], ALU.mult, ALU.add)
    onesA = P.sb("onesA", [128, 256]); P.memset("pool", onesA[:], 1.0)
    sp = {}
    for nm in ("a", "b"):
        T, np_, K, junk, lo, hi, mid, cnt, ge, d = st[nm]
        M = P.sb("M_" + nm, [np_, 256]); cum = P.sb("cum_" + nm, [np_, 256]); SP = P.sb("SP_" + nm, [np_, 256])
        SPi = P.sb("SPi_" + nm, [np_, 256], I32)
        P.ts("dve", M[:], T[:], lo[:, 0:1], ALU.is_ge)
        P.op("dve", lambda eng, cum=cum, M=M, np_=np_: eng.tensor_tensor_scan(cum[:], onesA[:np_, :], M[:], 0.0, ALU.mult, ALU.add),
             reads=[onesA[:np_, :], M[:]], writes=[cum[:]])
        if nm == "a":
            P.matmul(pcnt[:, 1:2], LT[:], cum[:, 255:256])
            P.ts("dve", SP[:], cum[:], pcnt[:, 1:2], ALU.add, -1.0 - K, ALU.add)
        else:
            P.ts("dve", SP[:], cum[:], -1.0 - K, ALU.add)
        P.tt("dve", SP[:], SP[:], M[:], ALU.mult)
        P.ts("dve", SP[:], SP[:], float(K), ALU.add)
        P.ts("dve", SP[:], SP[:], float(K), ALU.min)
        P.copy("dve", SPi[:], SP[:])
        sp[nm] = SP
        P.dma(sposA_d[:, :] if nm == "a" else sposB_d[:, :], SPi[:])
    pT = P.ps("pT", [128, 8, 128])
    TA = P.sb("TA", [128, 2, 2, 128])
    for qi, src in enumerate((sp["a"], A)):
        for fi in range(2):
            P.transpose(pT[:, qi * 2 + fi, :], src[:, fi * 128:(fi + 1) * 128], ident[:])
            P.copy("dve", TA[:, qi, fi, :], pT[:, qi * 2 + fi, :])
    TB = P.sb("TB", [128, 2, 2, 4])
    for qi, src in enumerate((sp["b"], B)):
        for fi in range(2):
            P.transpose(pT[:, 4 + qi * 2 + fi, 0:4], src[:, fi * 128:(fi + 1) * 128], ident[:4, :4])
            P.copy("dve", TB[:, qi, fi, :], pT[:, 4 + qi * 2 + fi, 0:4])
    R3 = P.sb("R3", [128, 2, 4, 32, 3])
    for fi in range(2):
        for r in range(4):
            P.copy("pool", R3[:, fi, r, :, 0:2], tokc[:, fi, :, :])
            P.copy("pool", R3[:, fi, r, :, 2], TA[:, 1, fi, r * 32:(r + 1) * 32])
    R3c = P.sb("R3c", [128, 2, 4, 3])
    for fi in range(2):
        for r in range(4):
            P.copy("pool", R3c[:, fi, r, 0:2], tokcc[:, fi, :])
            P.copy("pool", R3c[:, fi, r, 2:3], TB[:, 1, fi, r:r + 1])
    pidx = P.ps("pidx", [128, 512])
    pa = P.ps("pa", [128, 512]); pb = P.ps("pb", [128, 512])
    OH = [P.sb("OH%d" % i, [128, 1024]) for i in range(2)]
    RG = P.sb("RG", [3, 1024])
    n = 0
    for r in range(4):
        for tile in range(64):
            pp, fi = tile // 2, tile % 2
            O = OH[n % 2]; n += 1
            P.ts("dve" if n % 2 else "pool", O[:], iota[:], TA[:, 0, fi, r * 32 + pp:r * 32 + pp + 1], ALU.is_equal)
            P.matmul(pa[0:3, :], R3[:, fi, r, pp, :], O[:, 0:512], start=(tile == 0), stop=(tile == 63))
            P.matmul(pb[0:3, :], R3[:, fi, r, pp, :], O[:, 512:1024], start=(tile == 0), stop=(tile == 63))
        P.copy("dve", RG[:, 0:512], pa[0:3, :]); P.copy("act", RG[:, 512:1024], pb[0:3, :])
        for stl in range(8):
            c0 = (r * 8 + stl) * 3
            P.transpose(pidx[:, c0:c0 + 3], RG[0:3, stl * 128:(stl + 1) * 128], ident[0:3, 0:3])
    OHc = [P.sb("OHc%d" % i, [128, 32]) for i in range(2)]
    RGc = P.sb("RGc", [3, 32])
    for r in range(4):
        for tile in range(2):
            O = OHc[n % 2]; n += 1
            P.ts("dve", O[:], iota[:, 0:32], TB[:, 0, tile, r:r + 1], ALU.is_equal)
            P.matmul(pa[0:3, 0:32], R3c[:, tile, r, :], O[:], start=(tile == 0), stop=(tile == 1))
        P.copy("dve", RGc[:], pa[0:3, 0:32])
        P.transpose(pidx[:32, 128 + r * 3:128 + r * 3 + 3], RGc[0:3, :], ident[0:3, 0:3])
    IG = P.sb("IG", [128, 4, 8, 3]); IGc = P.sb("IGc", [32, 4, 3])
    P.copy("dve", IG[:].rearrange("p a b c -> p (a b c)"), pidx[:, 0:96])
    P.copy("dve", IGc[:].rearrange("p a c -> p (a c)"), pidx[:32, 128:140])
    idxf = P.sb("idxf", [128, 4, 8]); idxi = P.sb("idxi", [128, 4, 8], I32)
    P.ts("dve", idxf[:], IG[:, :, :, 0], 64.0, ALU.mult); P.tt("dve", idxf[:], idxf[:], IG[:, :, :, 1], ALU.add)
    P.copy("dve", idxi[:], idxf[:])
    idxcf = P.sb("idxcf", [32, 4]); idxci = P.sb("idxci", [32, 4], I32)
    P.ts("dve", idxcf[:], IGc[:, :, 0], 64.0, ALU.mult); P.tt("dve", idxcf[:], idxcf[:], IGc[:, :, 1], ALU.add)
    P.copy("dve", idxci[:], idxcf[:])
    if dbg == 1:
        dbg_d = P.dram("dbg_idx", [128, 32], I32, "ExternalOutput")
        P.dma(dbg_d[:, :], idxi[:].rearrange("p a b -> p (a b)"))
        dbg2_d = P.dram("dbg_gate", [128, 4, 8, 3], F32, "ExternalOutput")
        P.dma(dbg2_d[:, :, :, :], IG[:])
        return P.finish()
    W1 = P.sb("W1", [128, 8, 2048], BF16); W3 = P.sb("W3", [128, 8, 2048], BF16); W2 = P.sb("W2", [128, 16, 1024], BF16)
    stage = [P.sb("stg%d" % i, [128, 8, 512]) for i in range(2)]
    XS = [P.sb("XS%d" % i, [128, 1024], BF16) for i in range(4)]
    for t in XS:
        P.memset("pool", t[:], 0.0)
    xsT = P.sb("xsT", [128, 8, 512], BF16)
    h1T = P.sb("h1T", [128, 16, 512], BF16)
    pTb = P.ps("pTb", [128, 8, 128], BF16)
    pTf = pT[:].rearrange("p a b -> p (a b)"); py = [pTf[:, i * 512:(i + 1) * 512] for i in range(2)]
    sl = [P.sb("sl%d" % i, [128, 512]) for i in range(2)]
    yo = [P.sb("yo%d" % i, [128, 1024], BF16) for i in range(2)]
    nx = 0
    for j in range(2):
        load_w_bf16(P, w1_d[j], W1, 1024, 2048, stage)
        load_w_bf16(P, w3_d[j], W3, 1024, 2048, stage)
        load_w_bf16(P, w2_d[j], W2, 2048, 1024, stage)
        blocks = []
        for s in range(2):
            for b in range(2):
                blocks.append(("lat", s, b * 4, 4, 128))
        for s in range(2):
            blocks.append(("ctx", s, 0, 1, 32))
        for (kind, s, st0, nst, rows) in blocks:
            r = j * 2 + s
            bn = nst * rows
            for a in range(nst):
                X = XS[nx % 4]; nx += 1
                if kind == "lat":
                    P.idma(X[:rows, :], hf_d[s][:, :], idxi[:, r, st0 + a:st0 + a + 1], 8191)
                else:
                    P.idma(X[:rows, :], hfc_d[s][:, :], idxci[:, r:r + 1], 255)
                for k in range(8):
                    P.transpose(pTb[:, k, :rows], X[:rows, k * 128:(k + 1) * 128], identb[:rows, :rows])
                P.copy("dve", xsT[:, 0:4, a * rows:(a + 1) * rows], pTb[:, 0:4, :rows])
                P.copy("act", xsT[:, 4:8, a * rows:(a + 1) * rows], pTb[:, 4:8, :rows])
            for f in range(16):
                pa_ = pa if f % 2 == 0 else pcnt
                pb_ = pb if f % 2 == 0 else pidx
                for k in range(8):
                    P.matmul(pa_[:, :bn], W1[:, k, f * 128:(f + 1) * 128], xsT[:, k, :bn], start=(k == 0), stop=(k == 7))
                for k in range(8):
                    P.matmul(pb_[:, :bn], W3[:, k, f * 128:(f + 1) * 128], xsT[:, k, :bn], start=(k == 0), stop=(k == 7))
                S_ = sl[f % 2]
                P.act(S_[:, :bn], pa_[:, :bn], AF.Silu)
                P.tt("dve", h1T[:, f, :bn], S_[:, :bn], pb_[:, :bn], ALU.mult)
            for a in range(nst):
                Y = yo[a % 2]
                for cb in range(2):
                    for f in range(16):
                        P.matmul(py[cb][:rows, :], h1T[:, f, a * rows:(a + 1) * rows], W2[:, f, cb * 512:(cb + 1) * 512], start=(f == 0), stop=(f == 15))
                    g = IG[:, r, st0 + a, 2:3] if kind == "lat" else IGc[:, r, 2:3]
                    if cb == 0:
                        P.ts("dve", Y[:rows, 0:512], py[cb][:rows, :], g[:rows], ALU.mult)
                    else:
                        P.act(Y[:rows, 512:1024], py[cb][:rows, :], AF.Copy, scale=g[:rows])
                if kind == "lat":
                    P.dma(ye_d[r, (st0 + a) * 128:(st0 + a + 1) * 128, :], Y[:rows, :], q="sp")
                else:
                    P.dma(yec_d[r, :, :], Y[:rows, :], q="sp")
    return P.finish()


def build_L4(final=False):
    NT = 2112
    P = Prog()
    ye_d = P.dram("ye_s", [16, CAP, 1024], BF16, "ExternalInput")
    yec_d = P.dram("yec_s", [16, CAPC, 1024], BF16, "ExternalInput")
    spos_d = P.dram("sposbc", [16, 128, NT], I32, "ExternalInput")
    slot_d = P.dram("slotid", [128, 8, 128], F32, "ExternalInput")
    xm_d = P.dram("x_mid", [NT, 1024], F32, "ExternalInput")
    gf_d = P.dram("gf", [128, 2, 1024], F32, "ExternalInput")
    out_d = P.dram("x_new", [NT, 1024], F32, "ExternalOutput")
    gf = P.sb("gf_sb", [128, 2, 1024]); P.dma(gf[:], gf_d[:, :, :])
    slot = P.sb("slot_sb", [128, 8, 128]); P.dma(slot[:], slot_d[:, :, :], q="act")
    if final:
        fn_d = P.dram("fnw", [128, 1024], F32, "ExternalInput")
        fnw = P.sb("fnw_sb", [128, 1024]); P.dma(fnw[:], fn_d[:, :], q="act")
        junk = P.sb("junk", [128, 1024]); ss = P.sb("ss", [128, 1]); tmp = P.sb("tmp", [128, 1]); rstd = P.sb("rstd", [128, 1])
    tiles = [(i * 128, 128) for i in range(16)] + [(2048, 64)]
    acc = [P.sb("acc%d" % i, [128, 1024]) for i in range(17)]
    YE = [P.sb("YE%d" % i, [128, 8, 1024], BF16) for i in range(2)]
    YEc = [P.sb("YEc%d" % i, [32, 1024], BF16) for i in range(2)]
    SPi = [P.sb("SPi%d" % i, [128, NT], I32) for i in range(2)]
    SPf = [P.sb("SPf%d" % i, [128, NT]) for i in range(2)]
    OHT = [P.sb("OHT%d" % i, [128, 8, 128], BF16) for i in range(3)]
    pc = [P.ps("pc%d" % i, [128, 512]) for i in range(4)]
    n = 0
    for e in range(16):
        Y = YE[e % 2]; Yc = YEc[e % 2]; SI = SPi[e % 2]; SF = SPf[e % 2]
        P.dma(Y[:], ye_d[e].rearrange("(c p) d -> p c d", p=128), q="sp")
        P.dma(Yc[:], yec_d[e, :, :], q="act")
        P.dma(SI[:], spos_d[e, :, :], q="act")
        P.copy("pool", SF[:], SI[:])
        for ti, (t0, tr) in enumerate(tiles):
            O = OHT[n % 3]
            lat = t0 < 2048
            if lat:
                in0 = SF[:, t0:t0 + 128].unsqueeze(1).to_broadcast([128, 8, 128])
                P.tt("dve", O[:], in0, slot[:], ALU.is_equal)
            else:
                P.tt("dve", O[:32, 0, :64], SF[:32, t0:t0 + 64], slot[:32, 0, :64], ALU.is_equal)
            for cb in range(2):
                p = pc[(n % 2) * 2 + cb]
                if lat:
                    for c in range(8):
                        P.matmul(p[:, :], O[:, c, :], Y[:, c, cb * 512:(cb + 1) * 512], start=(c == 0), stop=(c == 7))
                else:
                    P.matmul(p[:64, :], O[:32, 0, :64], Yc[:, cb * 512:(cb + 1) * 512])
                A = acc[ti]
                if e == 0:
                    P.copy("act", A[:tr, cb * 512:(cb + 1) * 512], p[:tr, :])
                else:
                    P.tt("dve", A[:tr, cb * 512:(cb + 1) * 512], A[:tr, cb * 512:(cb + 1) * 512], p[:tr, :], ALU.add)
            n += 1
    xm = [P.sb("xm%d" % i, [128, 1024]) for i in range(2)]
    for ti, (t0, tr) in enumerate(tiles):
        XM = xm[ti % 2]; AC = acc[ti]
        v = 0 if t0 < 2048 else 1
        P.dma(XM[:tr, :], xm_d[t0:t0 + tr, :], q="act")
        P.tt("dve", AC[:tr, :], AC[:tr, :], gf[:tr, v, :], ALU.mult)
        P.tt("pool", AC[:tr, :], AC[:tr, :], XM[:tr, :], ALU.add)
        if final:
            rms_rstd(P, AC[:tr, :], tr, 1024, junk, ss, tmp, rstd)
            P.ts("dve", AC[:tr, :], AC[:tr, :], rstd[:tr, :], ALU.mult)
            P.tt("dve", AC[:tr, :], AC[:tr, :], fnw[:tr, :], ALU.mult)
        P.dma(out_d[t0:t0 + tr, :], AC[:tr, :], q="sp")
    return P.finish()


NEG = -30000.0


def load_w_bf16_p(P, w_dram, dst, K, N, pp, stage, cb=256):
    kc = K // pp
    wv = w_dram.rearrange("(c p) n -> p c n", p=pp)
    i = 0
    for k0 in range(0, kc, 8):
        for c0 in range(0, N, cb):
            st = stage[i % 2]
            P.dma(st[:pp, :8, :cb], wv[:, k0:k0 + 8, c0:c0 + cb], q="sp" if i % 2 == 0 else "act")
            P.copy(["dve", "pool", "act"][i % 3], dst[:, k0:k0 + 8, c0:c0 + cb], st[:pp, :8, :cb])
            i += 1


def build_L2_NA():
    P = Prog()
    C = Ctx()
    common_inputs(P, C)
    post_inputs(P, C)
    qT_d = P.dram("qT", [1024, NT], BF16, "ExternalInput")
    kT_d = P.dram("kT_loc", [1024, 2560], BF16, "ExternalInput")
    kTc_d = P.dram("kT_ctx", [1024, 256], BF16, "ExternalInput")
    vh_d = P.dram("vh", [16, 128, 20, 64], BF16, "ExternalInput")
    vc_d = P.dram("vc", [16, 128, 2, 64], BF16, "ExternalInput")
    bias_d = P.dram("biasT", [3, 16, 128, 6, 256], F32, "ExternalInput")
    wo_d = P.dram("wo", [1024, 1024], F32, "ExternalInput")
    S = mk_scratch(P)
    P.dma(S.ident[:], C.ident_d[:, :])
    wmf, shf = wmod_sh(P, C, "f")
    ones_b = P.sb("ones_b", [128, 64], BF16); P.memset("dve", ones_b[:], 1.0)
    wo_sb = P.sb("wo_sb", [64, 16, 1024], BF16)
    stage = [P.sb("stg%d" % i, [128, 8, 128]) for i in range(2)]
    load_w_bf16_p(P, wo_d, wo_sb, 1024, 1024, 64, stage, cb=128)
    onT = P.sb("onT", [64, 16, NT], BF16)
    K = P.sb("K", [64, 2560], BF16); Kc = P.sb("Kc", [64, 256], BF16); Q = P.sb("Q", [64, NT], BF16)
    V = P.sb("V", [128, 20, 64], BF16); Vc = P.sb("Vc", [128, 2, 64], BF16)
    bias = [P.sb("bias%d" % i, [128, 6, 256]) for i in range(2)]
    ps = [P.ps("ps%d" % i, [128, 512]) for i in range(3)]
    po = P.ps("po", [128, 512]); pd = P.ps("pd", [128, 512]); px = P.ps("px", [128, 512])
    sb_ = [P.sb("sb_%d" % i, [128, 256]) for i in range(2)]
    ET = [P.sb("ET%d" % i, [128, 256], BF16) for i in range(4)]
    r_ = P.sb("r_", [64, 256])
    n = 0
    nb = 0
    for h in range(16):
        P.dma(K[:], kT_d[h * 64:(h + 1) * 64, :], q="sp"); P.dma(Kc[:], kTc_d[h * 64:(h + 1) * 64, :], q="sp")
        P.dma(Q[:], qT_d[h * 64:(h + 1) * 64, :], q="act")
        P.dma(V[:], vh_d[h, :, :, :], q="sp"); P.dma(Vc[:], vc_d[h, :, :, :], q="act")
        for b in range(9):
            if b < 8:
                q0, qn = b * 256, 256
                tb = 0 if b == 0 else (2 if b == 7 else 1)
                Bs = bias[nb % 2]; nb += 1
                P.dma(Bs[:], bias_d[tb, h, :, :, :], q="sp" if nb % 2 else "act")
                chunks = [("w", j) for j in range(6)] + [("c", j) for j in range(2)]
            else:
                q0, qn = 2048, 64
                chunks = [("c", j) for j in range(2)]
            LA = 2

            def emit_qk(jj):
                kind, j = chunks[jj]
                if kind == "w":
                    lc = 2 * b + j
                    P.matmul(ps[(n + jj) % 3][:, :qn], K[:, lc * 128:(lc + 1) * 128], Q[:, q0:q0 + qn])
                else:
                    P.matmul(ps[(n + jj) % 3][:, :qn], Kc[:, j * 128:(j + 1) * 128], Q[:, q0:q0 + qn])
            for jj in range(min(LA, len(chunks))):
                emit_qk(jj)
            for ci, (kind, j) in enumerate(chunks):
                if ci + LA < len(chunks):
                    emit_qk(ci + LA)
                p = ps[(n + ci) % 3]; E = ET[(n + ci) % 4]; Sb = sb_[(n + ci) % 2]
                if kind == "w":
                    lc = 2 * b + j
                    P.stt("dve", Sb[:, :qn], p[:, :qn], 0.125, Bs[:, j, :qn], ALU.mult, ALU.add)
                    P.act(E[:, :qn], Sb[:, :qn], AF.Exp)
                    vv = V[:, lc, :]
                else:
                    P.act(E[:, :qn], p[:, :qn], AF.Exp, scale=0.125)
                    vv = Vc[:, j, :]
                P.matmul(po[:64, :qn], vv, E[:, :qn], start=(ci == 0), stop=(ci == len(chunks) - 1))
                P.matmul(pd[:64, :qn], ones_b[:], E[:, :qn], start=(ci == 0), stop=(ci == len(chunks) - 1))
            n += len(chunks)
            P.recip(r_[:, :qn], pd[:64, :qn])
            P.tt("dve", onT[:, h, q0:q0 + qn], po[:64, :qn], r_[:, :qn], ALU.mult)
    post_mixer(P, C, S, lambda k, t0, tr: onT[:, k, t0:t0 + tr], C.x_d, C.xmid_d, C.hf_d, C.aff_d, wo_sb, 16, C.bcp,
               wmf, shf, C.rw, [ps[0], ps[1]], px)
    return P.finish()


NU = 8448
NCH = 66


def bc_mid(ap2, n):
    return ap2.unsqueeze(1).to_broadcast([ap2.shape[0], n, ap2.shape[1]])


def bc_last(ap2, n):
    return ap2.unsqueeze(2).to_broadcast([ap2.shape[0], ap2.shape[1], n])


def build_L1_SSD(dbg=0):
    P = Prog()
    C = Ctx()
    common_inputs(P, C)
    x_d = P.dram("x_all", [NU, 1024], F32, "ExternalInput")
    w_d = P.dram("w_in_g", [1024, 1296], F32, "ExternalInput")
    cw_d = P.dram("cw", [128, 6, 3], F32, "ExternalInput")
    cb_d = P.dram("cb", [128, 6], F32, "ExternalInput")
    dtb_d = P.dram("dtb", [128, 16], F32, "ExternalInput")
    alog_d = P.dram("alog", [128, 16], F32, "ExternalInput")
    dsk_d = P.dram("dsk", [128, 16], F32, "ExternalInput")
    tri_d = P.dram("tri", [2, 128, 128], F32, "ExternalInput")
    neg_d = P.dram("neg", [2, 128, 128], F32, "ExternalInput")
    yz_d = P.dram("yz", [NU, 512], BF16, "ExternalOutput")
    preT_d = P.dram("preT", [768, NU], F32, "Internal")
    zs_d = P.dram("zs", [NU, 512], BF16, "Internal")
    y1_d = P.dram("y1s", [NU, 512], F32, "Internal")

    S = mk_scratch(P)
    P.dma(S.ident[:], C.ident_d[:, :])
    identb = P.sb("identb", [128, 128], BF16); P.copy("dve", identb[:], S.ident[:])
    wm, sh = wmod_sh(P, C, "m")
    cw = P.sb("cw_sb", [128, 6, 3]); cb = P.sb("cb_sb", [128, 6]); dtb = P.sb("dtb_sb", [128, 16])
    A = P.sb("A_sb", [128, 16]); dsk = P.sb("dsk_sb", [128, 16]); Dsum = P.sb("Dsum", [128, 8])
    tri = P.sb("tri_sb", [128, 2, 128]); neg = P.sb("neg_sb", [128, 2, 128])
    P.dma(cw[:], cw_d[:, :, :]); P.dma(cb[:], cb_d[:, :]); P.dma(dtb[:], dtb_d[:, :]); P.dma(A[:], alog_d[:, :], q="act")
    P.dma(dsk[:], dsk_d[:, :], q="act")
    for d in range(2):
        P.dma(tri[:, d, :], tri_d[d, :, :], q="act"); P.dma(neg[:, d, :], neg_d[d, :, :], q="act")
    P.act(A[:], A[:], AF.Exp)
    P.ts("dve", A[:], A[:], -1.0, ALU.mult)
    P.tt("dve", Dsum[:], dsk[:, 0:8], dsk[:, 8:16], ALU.add)
    AR = Arena(P, "arena", 68 * 1024)
    wsb = AR.alloc([128, 8, 1280], BF16)
    wdt = AR.alloc([128, 8, 16])
    stage = [AR.alloc([128, 8, 128]) for i in range(2)]
    wv = w_d.rearrange("(c p) n -> p c n", p=128)
    for i in range(10):
        st = stage[i % 2]
        P.dma(st[:], wv[:, :, i * 128:(i + 1) * 128], q="sp" if i % 2 == 0 else "act")
        P.copy(["dve", "pool", "act"][i % 3], wsb[:, :, i * 128:(i + 1) * 128], st[:])
    P.dma(wdt[:], wv[:, :, 1280:1296])
    x_tm = P.sb("x_tm", [128, NCH, 512], BF16)
    BT = P.sb("BT", [128, NU], BF16); CT = P.sb("CT", [128, NU], BF16)
    B_tm = P.sb("B_tm", [128, NCH, 128], BF16)
    dt = P.sb("dt", [128, NCH, 16])
    pk = [P.ps("pk%d" % i, [128, 512]) for i in range(6)]
    xt = [AR.alloc([128, 1024]) for i in range(2)]
    hT32 = AR.alloc([128, 8, 512]); hTb = AR.alloc([128, 8, 512], BF16)
    ev = [AR.alloc([128, 512]) for i in range(2)]
    zo = [AR.alloc([128, 512], BF16) for i in range(2)]
    spa = P.sb("spa", [128, 16]); spe = P.sb("spe", [128, 16]); spx = P.sb("spx", [128, 16])
    n = 0
    for b0 in range(0, NU, 512):
        bn = min(512, NU - b0)
        nt = bn // 128
        for a in range(nt):
            u0 = b0 + a * 128
            v = 1 if u0 < 256 else 0
            X = xt[n % 2]; n += 1
            P.dma(X[:], x_d[u0:u0 + 128, :], q="act")
            norm_T(P, S, X[:], 128, wm[v], sh[v], hT32, a * 128)
        P.copy("pool", hTb[:, :, :bn], hT32[:, :, :bn])
        for j in range(6):
            p = pk[j % 2]
            for k in range(8):
                P.matmul(p[:, :bn], wsb[:, k, 512 + j * 128:512 + (j + 1) * 128], hTb[:, k, :bn], start=(k == 0), stop=(k == 7))
            E = ev[j % 2]
            P.copy("act" if j % 2 == 0 else "dve", E[:, :bn], p[:, :bn])
            P.dma(preT_d[j * 128:(j + 1) * 128, b0:b0 + bn], E[:, :bn], q="sp")
        for a in range(nt):
            u0 = b0 + a * 128
            p = pk[2 + a % 2]
            for k in range(8):
                P.matmul(p[:, :], hTb[:, k, a * 128:(a + 1) * 128], wsb[:, k, 0:512], start=(k == 0), stop=(k == 7))
            Z = zo[a % 2]
            P.act(Z[:], p[:], AF.Silu)
            P.dma(zs_d[u0:u0 + 128, :], Z[:], q="sp")
            pd_ = pk[4]
            for k in range(8):
                P.matmul(pd_[:, 0:16], hT32[:, k, a * 128:(a + 1) * 128], wdt[:, k, :], start=(k == 0), stop=(k == 7))
            P.tt("dve", spx[:], pd_[:, 0:16], dtb[:], ALU.add)
            P.ts("dve", spa[:], spx[:], -1.0, ALU.mult)
            P.tt("dve", spa[:], spa[:], spx[:], ALU.max)
            P.act(spe[:], spa[:], AF.Exp, scale=-1.0)
            P.act(spe[:], spe[:], AF.Ln, bias=1.0)
            P.ts("dve", spx[:], spx[:], 0.0, ALU.max)
            P.tt("dve", dt[:, u0 // 128, :], spx[:], spe[:], ALU.add)
    CW = 2048
    P.barrier(); AR.reset()
    pre = [AR.alloc([128, CW + 2]) for i in range(2)]
    cv = [AR.alloc([128, CW]) for i in range(2)]
    cvb = [AR.alloc([128, CW], BF16) for i in range(2)]
    pTb3 = pk[5].bitcast(BF16)[:, 0:1024].rearrange("p (a b) -> p a b", b=128)
    n = 0
    segs = [(0, 256)] + [(256 + i * CW, CW) for i in range(4)]
    for j in range(6):
        for (u0, un) in segs:
            lo_edge = (u0 == 0 or u0 == 256)
            hi_edge = (u0 + un == 256 or u0 + un == NU)
            Pr = pre[n % 2]; Cv = cv[n % 2]; Cb = cvb[n % 2]; n += 1
            if lo_edge:
                P.memset("pool", Pr[:, 0:1], 0.0)
            if hi_edge:
                P.memset("pool", Pr[:, un + 1:un + 2], 0.0)
            a0 = u0 - (0 if lo_edge else 1); a1 = u0 + un + (0 if hi_edge else 1)
            P.dma(Pr[:, (1 if lo_edge else 0):(1 if lo_edge else 0) + (a1 - a0)], preT_d[j * 128:(j + 1) * 128, a0:a1], q="act")
            P.ts("dve", Cv[:, :un], Pr[:, 1:un + 1], cw[:, j, 1:2], ALU.mult, cb[:, j:j + 1], ALU.add)
            P.stt("dve", Cv[:, :un], Pr[:, 0:un], cw[:, j, 0:1], Cv[:, :un], ALU.mult, ALU.add)
            P.stt("dve", Cv[:, :un], Pr[:, 2:un + 2], cw[:, j, 2:3], Cv[:, :un], ALU.mult, ALU.add)
            if j < 4 or j == 4:
                P.act(Cb[:, :un], Cv[:, :un], AF.Silu)
                if j == 4:
                    P.copy("pool", BT[:, u0:u0 + un], Cb[:, :un])
                for c in range(un // 128):
                    ch = (u0 // 128) + c
                    pt = pTb3[:, (c % 8), :]
                    P.transpose(pt, Cb[:, c * 128:(c + 1) * 128], identb[:])
                    dst = x_tm[:, ch, j * 128:(j + 1) * 128] if j < 4 else B_tm[:, ch, :]
                    P.copy("dve" if c % 2 else "act", dst, pt)
            else:
                P.act(CT[:, u0:u0 + un], Cv[:, :un], AF.Silu)
    if dbg == 1:
        d1 = P.dram("dbg_xtm", [128, NCH, 512], BF16, "ExternalOutput"); P.dma(d1[:, :, :], x_tm[:])
        d2 = P.dram("dbg_BT", [128, NU], BF16, "ExternalOutput"); P.dma(d2[:, :], BT[:])
        d3 = P.dram("dbg_CT", [128, NU], BF16, "ExternalOutput"); P.dma(d3[:, :], CT[:])
        d4 = P.dram("dbg_dt", [128, NCH, 16], F32, "ExternalOutput"); P.dma(d4[:, :, :], dt[:])
        d5 = P.dram("dbg_Btm", [128, NCH, 128], BF16, "ExternalOutput"); P.dma(d5[:, :, :], B_tm[:])
        return P.finish()
    pacs = S.pT[:].rearrange("p a b -> p (a b)")
    pcb, py, pyo, pst, psm = pk[0], pk[1], pk[2], pk[3], pk[4]
    P.barrier(); AR.reset()
    hst = AR.alloc([128, 512]); hbf = AR.alloc([128, 512], BF16)
    a_ = AR.alloc([128, 8]); abc = AR.alloc([128, 8, 128])
    acsrow = AR.alloc([128, 8, 128]); acscol = AR.alloc([128, 8])
    seg = AR.alloc([128, 8, 128]); MT = AR.alloc([128, 8, 128], BF16)
    eacs = AR.alloc([128, 8]); e2 = AR.alloc([128, 8]); w_ = AR.alloc([128, 8]); cdec = AR.alloc([128, 8])
    xdd = AR.alloc([128, 512], BF16)
    t_ = AR.alloc([128, 512]); yv = [AR.alloc([128, 512]) for i in range(2)]
    y1l = [AR.alloc([128, 512]) for i in range(2)]
    zl = [AR.alloc([128, 512], BF16) for i in range(2)]
    yo = [AR.alloc([128, 512], BF16) for i in range(2)]
    for d in (1, 0):
        order = list(range(NCH)) if d == 0 else [1, 0] + list(range(NCH - 1, 1, -1))
        lend = 127 if d == 0 else 0
        P.memset("dve", hst[:], 0.0); P.memset("dve", hbf[:], 0.0)
        for ci, c in enumerate(order):
            u0 = c * 128
            dtc = dt[:, c, d * 8:(d + 1) * 8]
            P.tt("dve", a_[:], dtc, A[:, d * 8:(d + 1) * 8], ALU.mult)
            P.copy("pool", abc[:], bc_last(a_[:], 128))
            for r in range(8):
                P.matmul(pacs[:, r * 128:(r + 1) * 128], abc[:, r, :], tri[:, d, :])
            P.matmul(psm[:, 0:8], tri[:, d, :], a_[:])
            P.copy("act", acsrow[:].rearrange("p a b -> p (a b)"), pacs)
            P.copy("dve", acscol[:], psm[:, 0:8])
            P.tt("dve", seg[:], acsrow[:], bc_last(acscol[:], 128), ALU.subtract)
            P.tt("pool", seg[:], seg[:], bc_mid(neg[:, d, :], 8), ALU.add)
            P.act(seg[:].rearrange("p a b -> p (a b)"), seg[:].rearrange("p a b -> p (a b)"), AF.Exp)
            P.matmul(pcb[:, 0:128], BT[:, u0:u0 + 128], CT[:, u0:u0 + 128])
            P.tt("dve", seg[:], seg[:], bc_mid(pcb[:, 0:128], 8), ALU.mult)
            P.tt("dve", MT[:], seg[:], bc_last(dtc, 128), ALU.mult)
            for r in range(8):
                P.matmul(py[:, r * 64:(r + 1) * 64], MT[:, r, :], x_tm[:, c, r * 64:(r + 1) * 64])
            P.matmul(pyo[:, :], CT[:, u0:u0 + 128], hbf[:])
            P.act(eacs[:], acscol[:], AF.Exp)
            P.tt("dve", t_[:].rearrange("p (a b) -> p a b", b=64), pyo[:].rearrange("p (a b) -> p a b", b=64), bc_last(eacs[:], 64), ALU.mult)
            Y = yv[ci % 2]
            P.tt("dve", Y[:], t_[:], py[:], ALU.add)
            P.tt("dve", e2[:], acsrow[:, :, lend], acscol[:], ALU.subtract)
            P.act(e2[:], e2[:], AF.Exp)
            P.tt("dve", w_[:], e2[:], dtc, ALU.mult)
            P.tt("pool", xdd[:].rearrange("p (a b) -> p a b", b=64), x_tm[:, c, :].rearrange("p (a b) -> p a b", b=64), bc_last(w_[:], 64), ALU.mult)
            P.matmul(pst[:, :], B_tm[:, c, :], xdd[:])
            P.act(cdec[:], acsrow[:, :, lend], AF.Exp)
            P.tt("dve", hst[:].rearrange("p (a b) -> p a b", b=64), hst[:].rearrange("p (a b) -> p a b", b=64), bc_last(cdec[:], 64), ALU.mult)
            P.tt("dve", hst[:], hst[:], pst[:], ALU.add)
            P.copy("act", hbf[:], hst[:])
            if d == 1:
                P.dma(y1_d[u0:u0 + 128, :], Y[:], q="sp")
            else:
                Y1 = y1l[ci % 2]; Z = zl[ci % 2]; O = yo[ci % 2]
                P.dma(Y1[:], y1_d[u0:u0 + 128, :], q="sp"); P.dma(Z[:], zs_d[u0:u0 + 128, :], q="sp")
                P.tt("pool", Y[:], Y[:], Y1[:], ALU.add)
                P.tt("dve", t_[:].rearrange("p (a b) -> p a b", b=64), x_tm[:, c, :].rearrange("p (a b) -> p a b", b=64), bc_last(Dsum[:], 64), ALU.mult)
                P.tt("dve", Y[:], Y[:], t_[:], ALU.add)
                P.tt("dve", O[:], Y[:], Z[:], ALU.mult)
                P.dma(yz_d[u0:u0 + 128, :], O[:], q="sp")
    return P.finish()


def build_L2_SSD():
    P = Prog()
    C = Ctx()
    common_inputs(P, C)
    post_inputs(P, C)
    yz_d = P.dram("yz_rows", [NT, 2048], BF16, "ExternalInput")
    nw_d = P.dram("ssm_norm_bc", [128, 2048], F32, "ExternalInput")
    wo_d = P.dram("w_out", [2048, 1024], F32, "ExternalInput")
    S = mk_scratch(P)
    P.dma(S.ident[:], C.ident_d[:, :])
    identb = P.sb("identb", [128, 128], BF16); P.copy("dve", identb[:], S.ident[:])
    wmf, shf = wmod_sh(P, C, "f")
    nw = P.sb("nw_sb", [128, 2048]); P.dma(nw[:], nw_d[:, :])
    wo_sb = P.sb("wo_sb", [128, 16, 1024], BF16)
    stage = [P.sb("stg%d" % i, [128, 8, 128]) for i in range(2)]
    load_w_bf16(P, wo_d, wo_sb, 2048, 1024, stage, cb=128)
    ynT = P.sb("ynT", [128, 16, NT], BF16)
    yzt = [P.sb("yzt%d" % i, [128, 2048], BF16) for i in range(2)]
    yf = P.sb("yf", [128, 2048]); ynb = P.sb("ynb", [128, 2048], BF16)
    ss = P.sb("g_ss", [128, 1]); tmp = P.sb("g_tmp", [128, 1]); rstd = P.sb("g_rstd", [128, 1])
    pa = P.ps("pa", [128, 512]); pb = P.ps("pb", [128, 512]); px = P.ps("px", [128, 512])
    pTb = [P.ps("pTb%d" % i, [128, 8, 128], BF16) for i in range(2)]
    for ti, (t0, tr) in enumerate(TILES):
        Y = yzt[ti % 2]
        P.dma(Y[:tr, :], yz_d[t0:t0 + tr, :], q="act")
        P.act(yf[:tr, :], Y[:tr, :], AF.Square, accum_out=ss[:tr, :])
        P.ts("dve", tmp[:tr, :], ss[:tr, :], 1.0 / 2048, ALU.mult, EPS, ALU.add)
        P.act(tmp[:tr, :], tmp[:tr, :], AF.Sqrt)
        P.recip(rstd[:tr, :], tmp[:tr, :])
        P.ts("dve", yf[:tr, :], Y[:tr, :], rstd[:tr, :], ALU.mult)
        P.tt("pool", ynb[:tr, :], yf[:tr, :], nw[:tr, :], ALU.mult)
        for half in range(2):
            pt = pTb[half]
            for k in range(8):
                kk = half * 8 + k
                P.transpose(pt[:, k, :tr], ynb[:tr, kk * 128:(kk + 1) * 128], identb[:tr, :tr])
            P.copy("act" if half == 0 else "dve", ynT[:, half * 8:(half + 1) * 8, t0:t0 + tr], pt[:, :, :tr])
    post_mixer(P, C, S, lambda k, t0, tr: ynT[:, k, t0:t0 + tr], C.x_d, C.xmid_d, C.hf_d, C.aff_d, wo_sb, 16, C.bcp,
               wmf, shf, C.rw, [pa, pb], px)
    return P.finish()


_PROGS = {}


def _prog(name, fn, *a):
    key = (name,) + a
    if key not in _PROGS:
        _PROGS[key] = fn(*a)
    return _PROGS[key]


def _run(nc, ins):
    res = run_bass_kernel_spmd(nc, ins, core_ids=list(range(8)))
    return res.results


def _lambda_init(layer):
    import math
    return 0.8 - 0.6 * math.exp(-0.3 * layer)


def kernel(**inp):
    inp = {k: np.asarray(v) for k, v in inp.items()}
    x = inp["x"]; ctx = inp["ctx"]
    eye = np.eye(128, dtype=np.float32)
    cond = np.stack([inp["c"][0], inp["c"][1], inp["c_ctx"]], 0)
    condT = np.ascontiguousarray(cond.reshape(3, 8, 128).transpose(2, 1, 0))
    r0 = _run(_prog("L0", build_L0), [{"condT": condT, "ada_w": inp["ada_w"][i % 4],
                                       "ada_b3": np.ascontiguousarray(np.broadcast_to(inp["ada_b"][i % 4], (3, 6144)))} for i in range(8)])
    mods = [r0[l]["mods"] for l in range(4)]
    mconsts = moe_consts()
    sconsts = ssd_consts()
    xs = [core_rows(x, ctx, i) for i in range(8)]
    ia = ib = ic = 0
    out = None
    for layer in range(4):
        kind = layer % 3
        mp = [modpack(mods[layer], inp["norm_mix"][layer], inp["norm_ffn"][layer], i // 4) for i in range(8)]
        rw = np.ascontiguousarray(inp["router_w"][layer].reshape(8, 128, 16).transpose(1, 0, 2))

        def common(i):
            return {"ident": eye, "modp": mp[i][0], "bcp": np.ascontiguousarray(mp[i][1].transpose(1, 0, 2)),
                    "x_rows": xs[i], "router_w": rw}
        if kind in (0, 2):
            wqkv = inp["da_wqkv"][ia] if kind == 0 else inp["na_wqkv"][ic]
            ins = []
            for i in range(8):
                if kind == 0:
                    cosT, sinT = rope_tables(i)
                else:
                    cosT, sinT = np.ones((128, 2112), np.float32), np.zeros((128, 2112), np.float32)
                ins.append({"ident": eye, "modp": mp[i][0], "x_rows": xs[i], "wqkv": wqkv, "cosT": cosT, "sinT": sinT, "RT": rope_RT()})
            r1 = _run(_prog("L1", build_L1_DA, 0), ins)
            qT = [r1[i]["qT"] for i in range(8)]; kT = [r1[i]["kT"] for i in range(8)]; v = [r1[i]["v"] for i in range(8)]
            if kind == 0:
                li = _lambda_init(layer)
                lamv = np.stack([inp["da_lam_q1"][ia], inp["da_lam_k1"][ia], inp["da_lam_q2"][ia], inp["da_lam_k2"][ia]], 0)
                ins = []
                for i in range(8):
                    s = i // 4
                    cores = range(4 * s, 4 * s + 4)
                    kT_full = np.concatenate([kT[c][:, 2048:] for c in cores] + [kT[c][:, :2048] for c in cores], 1)
                    v_full = np.concatenate([v[c][2048:] for c in cores] + [v[c][:2048] for c in cores], 0)
                    vh = np.ascontiguousarray(v_full.reshape(66, 128, 8, 128).transpose(2, 1, 0, 3))
                    d = common(i)
                    d.update({"qT": qT[i], "kT_full": np.ascontiguousarray(kT_full), "vh": vh, "wo": inp["da_wo"][ia],
                              "lamv": np.ascontiguousarray(np.broadcast_to(lamv, (128, 4, 64))).astype(np.float32),
                              "laminit": np.ascontiguousarray(np.broadcast_to(np.array([li, 1 - li], np.float32), (128, 2))),
                              "subln": np.ascontiguousarray(inp["da_subln"][ia].reshape(128, 1)).astype(np.float32)})
                    ins.append(d)
                r2 = _run(_prog("L2DA", build_L2_DA, 0), ins)
                ia += 1
            else:
                ins = []
                for i in range(8):
                    d = common(i)
                    d.update({"wo": inp["na_wo"][ic], "biasT": na_bias_tables(inp["na_rpb"][ic], i % 4)})
                    ins.append(l2na_inputs(i, qT, kT, v, d))
                r2 = _run(_prog("L2NA", build_L2_NA), ins)
                ic += 1
        else:
            xs_s = [np.concatenate([xs[4 * s + q][:2048] for q in range(4)], 0) for s in range(2)]
            cs_s = [np.concatenate([xs[4 * s + q][2048:] for q in range(4)], 0) for s in range(2)]
            ins = [l1ssd_inputs(i, xs_s[i // 4], cs_s[i // 4], inp["ssm_w_in"][ib], inp["ssm_conv_w"][ib], inp["ssm_conv_b"][ib],
                                inp["ssm_dt_bias"][ib], inp["ssm_A_log"][ib], inp["ssm_D"][ib], mp[i][0], sconsts) for i in range(8)]
            r1 = _run(_prog("L1S", build_L1_SSD, 0), ins)
            yz = [r1[i]["yz"] for i in range(8)]
            ins = [l2ssd_inputs(i, yz, common(i), inp["ssm_norm"][ib], inp["ssm_w_out"][ib]) for i in range(8)]
            r2 = _run(_prog("L2S", build_L2_SSD), ins)
            ib += 1
        aff_lat = [np.concatenate([r2[c]["aff"][:2048] for c in range(4 * s, 4 * s + 4)], 0) for s in range(2)]
        aff_ctx = [np.concatenate([r2[c]["aff"][2048:] for c in range(4 * s, 4 * s + 4)], 0) for s in range(2)]
        hf_lat = [np.ascontiguousarray(np.concatenate([r2[c]["h_f"][:2048] for c in range(4 * s, 4 * s + 4)], 0)) for s in range(2)]
        hf_ctx = [np.ascontiguousarray(np.concatenate([r2[c]["h_f"][2048:] for c in range(4 * s, 4 * s + 4)], 0)) for s in range(2)]
        ins = [l3_inputs(i, aff_lat, aff_ctx, hf_lat, hf_ctx, inp["exp_w1"][layer], inp["exp_w3"][layer], inp["exp_w2"][layer], mconsts)
               for i in range(8)]
        r3 = _run(_prog("L3", build_L3, 0), ins)
        ye = [r3[c]["ye"] for c in range(8)]; yec = [r3[c]["yec"] for c in range(8)]
        spl, spc = spos_token_major([r3[c]["sposA"] for c in range(8)], [r3[c]["sposB"] for c in range(8)])
        final = layer == 3
        ins = [l4_inputs(i, ye, yec, spl, spc, r2[i]["x_mid"], mp[i][1], inp["final_norm"] if final else None) for i in range(8)]
        r4 = _run(_prog("L4", build_L4, final), ins)
        xs = [r4[i]["x_new"] for i in range(8)]
    out = np.stack([np.concatenate([xs[4 * s + q][:2048] for q in range(4)], 0) for s in range(2)], 0)
    return np.ascontiguousarray(out.astype(np.float32, copy=False))
```
